# Optimizing a Trainium2 kernel written in Bass

```python
import math
import jax, jax.numpy as jnp
from jax import lax
import numpy as np

D_MODEL = 2048
BATCH = 4
SEQ = 4096
DEPTH = 2

CHUNK = 64
HEAD_DIM = 128
N_HEADS_A = D_MODEL // (2 * HEAD_DIM)
N_HEADS_B = D_MODEL // (2 * HEAD_DIM)
D_A = N_HEADS_A * HEAD_DIM
D_B = N_HEADS_B * HEAD_DIM
N_IDX_HEADS = 16
IDX_DIM = 64
TOPK_MAX = 256
ROPE_THETA = 10000.0
SPARSE_Q_BLOCK = 64
SB_Q_BLOCK = 128
D_FF = 5632
N_EXPERTS = 8
TOP_K_EXPERTS = 2
D_EXPERT = 7168
LN_EPS = 1e-5
DN_ALPHA = (2.0 * DEPTH) ** 0.25
DN_BETA = (8.0 * DEPTH) ** -0.25
N_DENSE = (DEPTH + 1) // 2
N_MOE = DEPTH // 2

_SPLIT_SIZES = (D_A, D_A, D_A,
                N_IDX_HEADS * IDX_DIM, IDX_DIM, N_IDX_HEADS,
                D_B, D_B, D_B,
                D_MODEL, D_MODEL)
IN_COLS = sum(_SPLIT_SIZES)
_OFFSETS = tuple(int(o) for o in np.cumsum((0,) + _SPLIT_SIZES))
SPLIT_POINTS = _OFFSETS[1:-1]
_VALUE_GROUPS = (2, 8)

kernel_name = 'hybrid_dsa_stickbreak_moe_deepnorm'

F32 = jnp.float32


def layer_norm(x, g, b):
    xf = x.astype(F32)
    mu = jnp.mean(xf, axis=-1, keepdims=True)
    var = jnp.mean(jnp.square(xf - mu), axis=-1, keepdims=True)
    y = (xf - mu) * lax.rsqrt(var + LN_EPS) * g.astype(F32) + b.astype(F32)
    return y.astype(x.dtype)


def rope_tables(positions, dim):
    inv_freq = ROPE_THETA ** (-jnp.arange(0, dim, 2, dtype=F32) / dim)
    ang = positions.astype(F32)[..., None] * inv_freq
    return jnp.cos(ang), jnp.sin(ang)


def apply_rope(x, cos, sin):
    xf = x.astype(F32)
    x1, x2 = jnp.split(xf, 2, axis=-1)
    c = cos[:, :, None, :]
    s = sin[:, :, None, :]
    return jnp.concatenate([x1 * c - x2 * s, x2 * c + x1 * s], axis=-1).astype(x.dtype)


def to_blocks(a, blk):
    b, s = a.shape[:2]
    return jnp.moveaxis(a.reshape((b, s // blk, blk) + a.shape[2:]), 1, 0)


def from_blocks(a):
    a = jnp.moveaxis(a, 0, 1)
    return a.reshape((a.shape[0], a.shape[1] * a.shape[2]) + a.shape[3:])


def dsa_sparse_attention(q, k, v, q_idx, k_idx, w_idx):
    bsz, s_len, n_h, dh = q.shape
    n_sel = min(TOPK_MAX, s_len // 4)
    key_chunk = jnp.arange(s_len) // CHUNK
    k_flat = k.reshape(bsz, s_len, n_h * dh)
    v_flat = v.reshape(bsz, s_len, n_h * dh)
    gather = jax.vmap(lambda table, idx: table[idx])
    scale = 1.0 / math.sqrt(dh)
    k_idx_f = k_idx.astype(F32)

    def block(args):
        qb, qib, wb, start = args
        t = start + jnp.arange(SPARSE_Q_BLOCK)
        q_chunk = t // CHUNK
        admissible = key_chunk[None, :] <= q_chunk[:, None]
        logits = jnp.einsum('bqhd,bsd->bqhs', qib.astype(F32), k_idx_f)
        score = jnp.einsum('bqh,bqhs->bqs', wb.astype(F32), jax.nn.relu(logits))
        score = jnp.where(admissible[None], score, -jnp.inf)
        _, sel = lax.top_k(score, n_sel)
        valid = (sel // CHUNK) <= q_chunk[None, :, None]
        kg = gather(k_flat, sel).reshape(bsz, SPARSE_Q_BLOCK, n_sel, n_h, dh)
        vg = gather(v_flat, sel).reshape(bsz, SPARSE_Q_BLOCK, n_sel, n_h, dh)
        s = jnp.einsum('bqhd,bqnhd->bqhn', qb.astype(F32), kg.astype(F32)) * scale
        s = jnp.where(valid[:, :, None, :], s, -jnp.inf)
        p = jax.nn.softmax(s, axis=-1)
        o = jnp.einsum('bqhn,bqnhd->bqhd', p, vg.astype(F32))
        return o.astype(q.dtype)

    starts = jnp.arange(s_len // SPARSE_Q_BLOCK) * SPARSE_Q_BLOCK
    out = lax.map(block, (to_blocks(q, SPARSE_Q_BLOCK), to_blocks(q_idx, SPARSE_Q_BLOCK),
                          to_blocks(w_idx, SPARSE_Q_BLOCK), starts))
    return from_blocks(out)


def stick_breaking_attention(q, k, v):
    bsz, s_len, n_h, dh = q.shape
    key_pos = jnp.arange(s_len)
    scale = 1.0 / math.sqrt(dh)
    k_f = k.astype(F32)
    v_f = v.astype(F32)

    def block(args):
        qb, start = args
        t = start + jnp.arange(SB_Q_BLOCK)
        strict = (key_pos[None, :] < t[:, None])[None, None]
        z = jnp.einsum('bqhd,bshd->bhqs', qb.astype(F32), k_f) * scale
        log_keep = jnp.where(strict, jax.nn.log_sigmoid(-z), 0.0)
        later = lax.cumsum(log_keep, axis=3, reverse=True) - log_keep
        a = jnp.where(strict, jnp.exp(jax.nn.log_sigmoid(z) + later), 0.0)
        o = jnp.einsum('bhqs,bshd->bqhd', a, v_f)
        return o.astype(q.dtype)

    starts = jnp.arange(s_len // SB_Q_BLOCK) * SB_Q_BLOCK
    out = lax.map(block, (to_blocks(q, SB_Q_BLOCK), starts))
    return from_blocks(out)


def hybrid_mixer(x, cos_h, sin_h, cos_i, sin_i, w_in, w_proj_a, w_proj_b, w_out):
    bsz, s_len, _ = x.shape
    h = jnp.einsum('bsd,dc->bsc', x, w_in)
    qa, ka, va, qi, ki, wi, qb, kb, vb, ga, gb = jnp.split(h, SPLIT_POINTS, axis=-1)
    heads = lambda t, n, d: t.reshape(bsz, s_len, n, d)
    qa = apply_rope(heads(qa, N_HEADS_A, HEAD_DIM), cos_h, sin_h)
    ka = apply_rope(heads(ka, N_HEADS_A, HEAD_DIM), cos_h, sin_h)
    va = heads(va, N_HEADS_A, HEAD_DIM)
    qi = apply_rope(heads(qi, N_IDX_HEADS, IDX_DIM), cos_i, sin_i)
    ki = apply_rope(ki[:, :, None, :], cos_i, sin_i)[:, :, 0, :]
    o_a = dsa_sparse_attention(qa, ka, va, qi, ki, wi).reshape(bsz, s_len, D_A)
    o_b = stick_breaking_attention(heads(qb, N_HEADS_B, HEAD_DIM), heads(kb, N_HEADS_B, HEAD_DIM),
                                   heads(vb, N_HEADS_B, HEAD_DIM)).reshape(bsz, s_len, D_B)
    merged = (jax.nn.sigmoid(ga) * jnp.einsum('bsc,cd->bsd', o_a, w_proj_a)
              + jax.nn.sigmoid(gb) * jnp.einsum('bsc,cd->bsd', o_b, w_proj_b))
    return jnp.einsum('bsd,de->bse', merged, w_out)


def swiglu(x, w_gate, w_up, w_down):
    return (jax.nn.silu(x @ w_gate) * (x @ w_up)) @ w_down


def moe_swiglu(x, router_w, w_gate, w_up, w_down):
    bsz, s_len, d = x.shape
    xt = x.reshape(bsz * s_len, d)
    logits = (xt @ router_w).astype(F32)
    top_val, top_idx = lax.top_k(logits, TOP_K_EXPERTS)
    gates = jax.nn.softmax(top_val, axis=-1)
    comb = jnp.sum(jax.nn.one_hot(top_idx, N_EXPERTS, dtype=F32) * gates[..., None], axis=1)
    y = jnp.zeros((bsz * s_len, d), F32)
    for e in range(N_EXPERTS):
        y = y + comb[:, e:e + 1] * swiglu(xt, w_gate[e], w_up[e], w_down[e]).astype(F32)
    return y.astype(x.dtype).reshape(bsz, s_len, d)


def setup_inputs(seed: int = 0) -> dict:
    key = jax.random.key(seed)
    ks = jax.random.split(key, 24)

    def normal(k, shape, fan_in, scale=1.0):
        return jax.random.normal(k, shape, F32) * (scale * fan_in ** -0.5)

    def gain(k, shape):
        return 1.0 + 0.02 * jax.random.normal(k, shape, F32)

    def bias(k, shape):
        return 0.02 * jax.random.normal(k, shape, F32)

    x = jax.random.normal(ks[0], (BATCH, SEQ, D_MODEL), F32)
    offsets = jax.random.randint(ks[1], (BATCH, 1), 0, 4096, dtype=jnp.int32)
    positions = offsets + jnp.arange(SEQ, dtype=jnp.int32)[None, :]
    col_scale = np.ones((IN_COLS,), np.float32)
    for g in _VALUE_GROUPS:
        col_scale[_OFFSETS[g]:_OFFSETS[g + 1]] = DN_BETA
    w_in = normal(ks[2], (DEPTH, D_MODEL, IN_COLS), D_MODEL) * jnp.asarray(col_scale)
    return {
        'x': x,
        'positions': positions,
        'ln_in_g': gain(ks[3], (D_MODEL,)),
        'ln_in_b': bias(ks[4], (D_MODEL,)),
        'w_in': w_in,
        'w_proj_a': normal(ks[5], (DEPTH, D_A, D_MODEL), D_A, DN_BETA),
        'w_proj_b': normal(ks[6], (DEPTH, D_B, D_MODEL), D_B, DN_BETA),
        'w_out': normal(ks[7], (DEPTH, D_MODEL, D_MODEL), D_MODEL, DN_BETA),
        'ln_mix_g': gain(ks[8], (DEPTH, D_MODEL)),
        'ln_mix_b': bias(ks[9], (DEPTH, D_MODEL)),
        'ffn_w_gate': normal(ks[10], (N_DENSE, D_MODEL, D_FF), D_MODEL),
        'ffn_w_up': normal(ks[11], (N_DENSE, D_MODEL, D_FF), D_MODEL),
        'ffn_w_down': normal(ks[12], (N_DENSE, D_FF, D_MODEL), D_FF, DN_BETA),
        'router_w': normal(ks[13], (N_MOE, D_MODEL, N_EXPERTS), D_MODEL),
        'moe_w_gate': normal(ks[14], (N_MOE, N_EXPERTS, D_MODEL, D_EXPERT), D_MODEL),
        'moe_w_up': normal(ks[15], (N_MOE, N_EXPERTS, D_MODEL, D_EXPERT), D_MODEL),
        'moe_w_down': normal(ks[16], (N_MOE, N_EXPERTS, D_EXPERT, D_MODEL), D_EXPERT, DN_BETA),
        'ln_ffn_g': gain(ks[17], (DEPTH, D_MODEL)),
        'ln_ffn_b': bias(ks[18], (DEPTH, D_MODEL)),
    }


def reference(x, positions, ln_in_g, ln_in_b, w_in, w_proj_a, w_proj_b, w_out, ln_mix_g, ln_mix_b,
              ffn_w_gate, ffn_w_up, ffn_w_down, router_w, moe_w_gate, moe_w_up, moe_w_down,
              ln_ffn_g, ln_ffn_b):
    cos_h, sin_h = rope_tables(positions, HEAD_DIM)
    cos_i, sin_i = rope_tables(positions, IDX_DIM)
    x = layer_norm(x, ln_in_g, ln_in_b)
    for layer in range(DEPTH):
        mix = hybrid_mixer(x, cos_h, sin_h, cos_i, sin_i, w_in[layer], w_proj_a[layer],
                           w_proj_b[layer], w_out[layer])
        x = layer_norm(DN_ALPHA * x + mix, ln_mix_g[layer], ln_mix_b[layer])
        i = layer // 2
        if layer % 2 == 0:
            f = swiglu(x, ffn_w_gate[i], ffn_w_up[i], ffn_w_down[i])
        else:
            f = moe_swiglu(x, router_w[i], moe_w_gate[i], moe_w_up[i], moe_w_down[i])
        x = layer_norm(DN_ALPHA * x + f, ln_ffn_g[layer], ln_ffn_b[layer])
    return x
```

```python
import math
from contextlib import ExitStack

import numpy as np
import ml_dtypes
import concourse.bass as bass
import concourse.mybir as mybir
from concourse.bass_utils import run_bass_kernel_spmd

F32 = mybir.dt.float32
BF16 = mybir.dt.bfloat16
I32 = mybir.dt.int32
ALU = mybir.AluOpType
AF = mybir.ActivationFunctionType

D = 2048
KC = 16
NH = 8
DH = 128
NIH = 16
DI = 64
IN_COLS = 11344
OFF = dict(qa=0, ka=1024, va=2048, qi=3072, ki=4096, wi=4160, qb=4176, kb=5200, vb=6224, ga=7248, gb=9296)
D_FF = 5632
D_EXP = 7168
NE = 8
TOPK = 256
LN_EPS = 1e-5
ALPHA = (2.0 * 2) ** 0.25
SCALE = 1.0 / math.sqrt(DH)
NEG = -1.0e30
NDSEM = 6


class Trk:
    __slots__ = ("w", "r")

    def __init__(self):
        self.w = None
        self.r = {}


class Buf:
    __slots__ = ("t", "k")

    def __init__(self, t):
        self.t = t
        self.k = Trk()


class Prog:
    ENGS = ("pe", "act", "dve", "pool", "sp")

    def __init__(self, nc, es):
        self.nc = nc
        self.es = es
        self.ops = {e: [] for e in self.ENGS}
        self.cnt = {}
        self.known = {e: {} for e in self.ENGS}
        self.dma_i = {e: 0 for e in self.ENGS}
        self.sems = {}
        self.nops = 0

    def _sem(self, k):
        s = self.sems.get(k)
        if s is None:
            nm = k if isinstance(k, str) else f"d_{k[1]}_{k[2]}"
            s = self.es.enter_context(self.nc.semaphore("s_" + nm))
            self.sems[k] = s
        return s

    def _collect(self, eng, reads, writes):
        waits = {}

        def need(kv):
            if kv is None:
                return
            k, v = kv
            if k == eng and eng == "pe":
                return
            if waits.get(k, 0) < v:
                waits[k] = v

        for t in reads:
            need(t.w)
        for t in writes:
            need(t.w)
            for kv in t.r.items():
                need(kv)
        kn = self.known[eng]
        out = []
        for k, v in waits.items():
            if kn.get(k, 0) >= v:
                continue
            kn[k] = v
            out.append((k, v))
        return out

    def op(self, eng, fn, reads=(), writes=()):
        reads = [b.k for b in reads]
        writes = [b.k for b in writes]
        waits = self._collect(eng, reads, writes)
        v = self.cnt.get(eng, 0) + 1
        self.cnt[eng] = v
        self.ops[eng].append((waits, fn, (eng, 1), v))
        self.nops += 1
        for t in reads:
            if t.r.get(eng, 0) < v:
                t.r[eng] = v
        for t in writes:
            t.w = (eng, v)
            t.r = {}

    def dma(self, q, fn, reads=(), writes=()):
        reads = [b.k for b in reads]
        writes = [b.k for b in writes]
        i = self.dma_i[q]
        self.dma_i[q] = i + 1
        key = ("d", q, i % NDSEM)
        waits = self._collect(q, reads, writes)
        prev = 16 * (i // NDSEM)
        if prev > 0 and self.known[q].get(key, 0) < prev:
            self.known[q][key] = prev
            waits.append((key, prev))
        v = prev + 16
        self.cnt[key] = v
        self.ops[q].append((waits, fn, (key, 16), v))
        self.nops += 1
        for t in reads:
            if t.r.get(key, 0) < v:
                t.r[key] = v
        for t in writes:
            t.w = (key, v)
            t.r = {}

    def coll(self, fn, reads=(), writes=()):
        reads = [b.k for b in reads]
        writes = [b.k for b in writes]
        key = ("d", "cc", self.dma_i.get("cc", 0))
        self.dma_i["cc"] = self.dma_i.get("cc", 0) + 1
        waits = self._collect("pool", reads, writes)
        self.cnt[key] = 1
        self.ops["pool"].append((waits, fn, (key, 1), 1))
        self.nops += 1
        for t in reads:
            t.r[key] = 1
        for t in writes:
            t.w = (key, 1)
            t.r = {}

    def barrier(self):
        snap = dict(self.cnt)
        for e in self.ENGS:
            waits = []
            for k, v in snap.items():
                if k == e and e == "pe":
                    continue
                if self.known[e].get(k, 0) < v:
                    self.known[e][k] = v
                    waits.append((k, v))
            if waits:
                self.ops[e].append((waits, None, None, 0))

    def emit(self):
        nc = self.nc
        if not hasattr(self, "base_new"):
            self.base_new = {}
            self.base_old = {}
        targets = {e: set() for e in self.ENGS}
        for e in self.ENGS:
            for waits, fn, inc, v in self.ops[e]:
                for k, wv in waits:
                    self._sem(k)
                    if isinstance(k, str):
                        targets[k].add(wv)
                if inc is not None:
                    self._sem(inc[0])
        remap = {}
        for e in self.ENGS:
            newc = self.base_new.get(e, 0)
            m = {}
            for wv in targets[e]:
                if wv <= self.base_old.get(e, 0):
                    m[wv] = self.base_new.get(e, 0)
            for waits, fn, inc, v in self.ops[e]:
                if fn is None or inc is None or not isinstance(inc[0], str):
                    continue
                if v in targets[e]:
                    newc += 1
                    m[v] = newc
            remap[e] = m
            self.base_new[e] = newc
            self.base_old[e] = self.cnt.get(e, 0)
        sems = self.sems
        with nc.Block() as block:
            engmap = {"pe": block.tensor, "act": block.scalar, "dve": block.vector,
                      "pool": block.gpsimd, "sp": block.sync}

            def mk(e, oplist):
                tg = targets[e]

                def body(eng):
                    for waits, fn, inc, v in oplist:
                        for k, wv in waits:
                            eng.wait_ge(sems[k], remap[k][wv] if isinstance(k, str) else wv)
                        if fn is not None:
                            ins = fn(eng)
                            if isinstance(inc[0], str):
                                if v in tg:
                                    ins.then_inc(sems[inc[0]], 1)
                            else:
                                ins.then_inc(sems[inc[0]], inc[1])
                return body

            for e in self.ENGS:
                if self.ops[e]:
                    engmap[e](mk(e, self.ops[e]))
        self.ops = {e: [] for e in self.ENGS}


class _View:
    __slots__ = ("t", "k")

    def __init__(self, base, ap):
        self.t = ap
        self.k = base.k


class Rot:
    def __init__(self, bufs):
        self.bufs = bufs
        self.i = 0

    def next(self):
        b = self.bufs[self.i % len(self.bufs)]
        self.i += 1
        return b


class Builder:
    def __init__(self, S, layers, debug=False, ncores=8):
        self.ncores = ncores
        self.S = S
        self.T = S // 2
        self.NST = S // 512
        self.NSLOT = self.NST // 2
        self.layers = layers
        self.debug = debug
        self.nc = bass.Bass("TRN2", target_bir_lowering=False)
        self.ges = ExitStack()
        self.P = Prog(self.nc, self.ges)
        self.alt = 0
        self.phase_id = 0

    def din(self, name, shape, dt):
        return Buf(self.nc.dram_tensor(name, list(shape), dt, kind="ExternalInput").ap())

    def dscr(self, name, shape, dt):
        kind = "ExternalOutput" if self.debug else "Internal"
        return Buf(self.nc.dram_tensor(name, list(shape), dt, kind=kind).ap())

    def sb(self, es, name, shape, dt):
        return Buf(es.enter_context(self.nc.sbuf_tensor(f"p{self.phase_id}_{name}", list(shape), dt)))

    def ps(self, es, name, shape, dt):
        return Buf(es.enter_context(self.nc.psum_tensor(f"p{self.phase_id}_{name}", list(shape), dt)))

    def sbn(self, es, name, n, shape, dt):
        return Rot([self.sb(es, f"{name}{i}", shape, dt) for i in range(n)])

    def psn(self, es, name, n, shape, dt):
        return Rot([self.ps(es, f"{name}{i}", shape, dt) for i in range(n)])

    def mm(self, ob, oap, lhsT, rhs, start, stop, rd):
        self.P.op("pe", lambda e: e.matmul(oap, lhsT, rhs, start=start, stop=stop, skip_group_check=True),
                  reads=rd, writes=[ob])

    def tp(self, ob, oap, in_ap, ident, rd):
        self.P.op("pe", lambda e: e.transpose(oap, in_ap, ident.t[:]), reads=rd + [ident], writes=[ob])

    def actf(self, ob, oap, iap, func, rd, bias=None, scale=None):
        kw = {}
        if bias is not None:
            kw["bias"] = bias
        if scale is not None:
            kw["scale"] = scale
        self.P.op("act", lambda e: e.activation(out=oap, in_=iap, func=func, **kw), reads=rd, writes=[ob])

    def copy(self, eng, ob, oap, iap, rd):
        if eng == "act":
            self.P.op("act", lambda e: e.activation(out=oap, in_=iap, func=AF.Copy), reads=rd, writes=[ob])
        else:
            self.P.op(eng, lambda e: e.tensor_copy(out=oap, in_=iap), reads=rd, writes=[ob])

    def altcopy(self, ob, oap, iap, rd):
        self.alt += 1
        self.copy("act" if self.alt % 2 else "dve", ob, oap, iap, rd)

    def tt(self, ob, oap, a, b, op, rd, eng="dve"):
        self.P.op(eng, lambda e: e.tensor_tensor(out=oap, in0=a, in1=b, op=op), reads=rd, writes=[ob])

    def ts(self, ob, oap, a, s1, s2, op0, op1, rd, eng="dve"):
        if s2 is None:
            self.P.op(eng, lambda e: e.tensor_scalar(out=oap, in0=a, scalar1=s1, scalar2=None, op0=op0),
                      reads=rd, writes=[ob])
        else:
            self.P.op(eng, lambda e: e.tensor_scalar(out=oap, in0=a, scalar1=s1, scalar2=s2, op0=op0, op1=op1),
                      reads=rd, writes=[ob])

    def stt(self, ob, oap, a, s, b, op0, op1, rd, eng="dve"):
        self.P.op(eng, lambda e: e.scalar_tensor_tensor(out=oap, in0=a, scalar=s, in1=b, op0=op0, op1=op1),
                  reads=rd, writes=[ob])

    def load(self, ob, oap, src, sap, q="sp"):
        self.P.dma(q, lambda e: e.dma_start(out=oap, in_=sap), reads=[src], writes=[ob])

    def store(self, dst, dap, sbuf, sap, q="sp"):
        self.P.dma(q, lambda e: e.dma_start(out=dap, in_=sap), reads=[sbuf], writes=[dst])

    def end_phase(self):
        self.P.barrier()
        self.P.emit()
        self.phase_id += 1

    def make_consts(self, es):
        nc, P = self.nc, self.P
        c = {}
        idf = self.sb(es, "idf", [128, 128], F32)
        P.op("pool", lambda e: e.memset(idf.t[:], 1.0), writes=[idf])
        P.op("pool", lambda e: e.affine_select(out=idf.t[:], in_=idf.t[:], pattern=[[-1, 128]],
                                               compare_op=ALU.is_equal, fill=0.0, base=0, channel_multiplier=1),
             reads=[idf], writes=[idf])
        idb = self.sb(es, "idb", [128, 128], BF16)
        self.copy("dve", idb, idb.t[:], idf.t[:], [idf])
        ones = self.sb(es, "onesb", [128, 128], BF16)
        P.op("dve", lambda e: e.memset(ones.t[:], 1.0), writes=[ones])
        nuf = self.sb(es, "nuf", [128, 128], F32)
        P.op("pool", lambda e: e.memset(nuf.t[:], -1.0), writes=[nuf])
        P.op("pool", lambda e: e.affine_select(out=nuf.t[:], in_=nuf.t[:], pattern=[[-1, 128]],
                                               compare_op=ALU.is_ge, fill=0.0, base=0, channel_multiplier=1),
             reads=[nuf], writes=[nuf])
        negu = self.sb(es, "negu", [128, 128], BF16)
        self.copy("dve", negu, negu.t[:], nuf.t[:], [nuf])
        c["idb"], c["ones"], c["negu"], c["idf"] = idb, ones, negu, idf
        return c

    def layer_norm(self, xin, xap, gB, bB, out32, o32ap, out16, o16ap, tmp):
        P = self.P
        st, mv, rs = tmp["st"], tmp["mv"], tmp["rs"]
        for j in range(4):
            P.op("dve", (lambda j: lambda e: e.bn_stats(st.t[:, 6 * j:6 * j + 6], xap[:, 512 * j:512 * j + 512]))(j),
                 reads=[xin], writes=[st])
        P.op("dve", lambda e: e.bn_aggr(mv.t[:], st.t[:]), reads=[st], writes=[mv])
        self.actf(rs, rs.t[:, 0:1], mv.t[:, 1:2], AF.Sqrt, [mv], bias=tmp["eps"].t[:, 0:1])
        P.op("dve", lambda e: e.reciprocal(out=rs.t[:, 0:1], in_=rs.t[:, 0:1]), reads=[rs], writes=[rs])
        self.stt(rs, rs.t[:, 1:2], mv.t[:, 0:1], -1.0, rs.t[:, 0:1], ALU.mult, ALU.mult, [mv, rs])
        y = tmp["y"]
        self.actf(y, y.t[:], xap, AF.Identity, [xin, rs], bias=rs.t[:, 1:2], scale=rs.t[:, 0:1])
        self.tt(y, y.t[:], y.t[:], gB.t[:], ALU.mult, [y, gB])
        if out32 is not None:
            self.tt(out32, o32ap, y.t[:], bB.t[:], ALU.add, [y, bB])
            if out16 is not None:
                self.copy("act", out16, o16ap, o32ap, [out32])
        else:
            self.tt(out16, o16ap, y.t[:], bB.t[:], ALU.add, [y, bB])

    def ln_tmp(self, es, pfx):
        t = {"st": self.sb(es, pfx + "st", [128, 24], F32), "mv": self.sb(es, pfx + "mv", [128, 2], F32),
             "rs": self.sb(es, pfx + "rs", [128, 2], F32), "y": self.sb(es, pfx + "y", [128, D], F32),
             "eps": self.sb(es, pfx + "eps", [128, 1], F32)}
        self.P.op("dve", lambda e: e.memset(t["eps"].t[:], LN_EPS), writes=[t["eps"]])
        return t

    def bcast_load(self, es, name, src, sap):
        b = self.sb(es, name, [128, D], F32)
        self.load(b, b.t[:], src, sap.partition_broadcast(128))
        return b

    def phase0(self, xs, xo, with_ln, lng, lnb, xnT_s, xnT_o, xres):
        with ExitStack() as es:
            C = self.make_consts(es)
            if with_ln:
                gB = self.bcast_load(es, "p0g", lng, lng.t)
                bB = self.bcast_load(es, "p0b", lnb, lnb.t)
                tmp = self.ln_tmp(es, "p0")
            xin = self.sbn(es, "p0x", 2, [128, D], F32)
            y32 = self.sbn(es, "p0y32", 2, [128, D], F32)
            y16 = self.sbn(es, "p0y16", 2, [128, D], BF16)
            xT = self.sbn(es, "p0xT", 2, [128, KC, 512], BF16)
            tps = self.psn(es, "p0tp", 2, [128, D], BF16)
            for (src, ntile, dstT, own) in ((xs, self.S // 128, xnT_s, False), (xo, self.T // 128, xnT_o, True)):
                for ti in range(ntile):
                    xi = xin.next()
                    self.load(xi, xi.t[:], src, src.t[ti * 128:(ti + 1) * 128, :])
                    b16 = y16.next()
                    if with_ln:
                        b32 = y32.next() if own else None
                        self.layer_norm(xi, xi.t[:], gB, bB, b32, b32.t[:] if own else None, b16, b16.t[:], tmp)
                        if own:
                            self.store(xres, xres.t[ti * 128:(ti + 1) * 128, :], b32, b32.t[:], q="pool")
                    else:
                        self.copy("act", b16, b16.t[:], xi.t[:], [xi])
                        if own:
                            self.store(xres, xres.t[ti * 128:(ti + 1) * 128, :], xi, xi.t[:])
                    sub = ti % 4
                    if sub == 0:
                        xTb = xT.next()
                    tpp = tps.next()
                    for k in range(KC):
                        self.tp(tpp, tpp.t[:, k * 128:(k + 1) * 128], b16.t[:, k * 128:(k + 1) * 128], C["idb"], [b16])
                    self.altcopy(xTb, xTb.t[:, :, sub * 128:(sub + 1) * 128],
                                 tpp.t[:].rearrange("p (k t) -> p k t", k=KC), [tpp])
                    if sub == 3:
                        st = ti // 4
                        self.store(dstT, dstT.t[st], xTb, xTb.t[:], q="pool")
            self.end_phase()

    def rope_tables(self, es, pfx):
        r = {"pi": self.sb(es, pfx + "pi", [128, 1], I32), "pf": self.sb(es, pfx + "pf", [128, 1], F32),
             "ang": self.sb(es, pfx + "ang", [128, 192], F32), "tab": self.sb(es, pfx + "tab", [128, 192], F32),
             "tmp": self.sb(es, pfx + "tmp", [128, 192], F32)}
        return r

    def compute_rope(self, r, frB, possrc, posap, tabB=None, tabap=None):
        P = self.P
        MAGIC = 12582912.0
        self.load(r["pi"], r["pi"].t[:], possrc, posap)
        self.copy("dve", r["pf"], r["pf"].t[:], r["pi"].t[:], [r["pi"]])
        ang, tmp, tab = r["ang"], r["tmp"], r["tab"]
        self.ts(ang, ang.t[:], frB.t[:], r["pf"].t[:, 0:1], None, ALU.mult, None, [frB, r["pf"]])
        for (a, b) in ((0, 64), (128, 160)):
            self.ts(ang, ang.t[:, a:b], ang.t[:, a:b], float(np.pi / 2), None, ALU.add, None, [ang])
        self.ts(tmp, tmp.t[:], ang.t[:], float(1 / (2 * np.pi)), MAGIC, ALU.mult, ALU.add, [ang])
        self.ts(tmp, tmp.t[:], tmp.t[:], MAGIC, float(-2 * np.pi), ALU.subtract, ALU.mult, [tmp])
        self.tt(tmp, tmp.t[:], tmp.t[:], ang.t[:], ALU.add, [tmp, ang])
        self.ts(tmp, tmp.t[:], tmp.t[:], float(-np.pi), float(np.pi), ALU.max, ALU.min, [tmp])
        if tabB is None:
            self.actf(tab, tab.t[:], tmp.t[:], AF.Sin, [tmp])
        else:
            self.actf(tabB, tabap, tmp.t[:], AF.Sin, [tmp])

    def rope_apply(self, ps, psap, nh, hd, cosap, sinap, out, oap, t1, t2, extra_scale=None, rd=()):
        h2 = hd // 2
        x = psap.rearrange("p (h t d) -> p h t d", h=nh, t=2)
        o = oap.rearrange("p (h t d) -> p h t d", h=nh, t=2)
        x1, x2 = x[:, :, 0, :], x[:, :, 1, :]
        cb = cosap.unsqueeze(1).to_broadcast([128, nh, h2])
        sb_ = sinap.unsqueeze(1).to_broadcast([128, nh, h2])
        a1 = t1.t[:, 0:nh * h2].rearrange("p (h d) -> p h d", h=nh)
        a2 = t2.t[:, 0:nh * h2].rearrange("p (h d) -> p h d", h=nh)
        rd = list(rd)
        self.tt(t1, a1, x1, cb, ALU.mult, [ps] + rd)
        self.tt(t2, a2, x2, sb_, ALU.mult, [ps] + rd)
        self.tt(out, o[:, :, 0, :], a1, a2, ALU.subtract, [t1, t2])
        self.tt(t1, a1, x2, cb, ALU.mult, [ps] + rd)
        self.tt(t2, a2, x1, sb_, ALU.mult, [ps] + rd)
        self.tt(out, o[:, :, 1, :], a1, a2, ALU.add, [t1, t2])

    def phaseA(self, L, w_in, xnT_s, xs_ap, xnT_o, xo_ap, pos_s, pos_o, frq, sc):
        nc, P = self.nc, self.P
        wv = w_in.t.rearrange("(k p) c -> p k c", p=128)
        NTS, NTO = self.S // 128, self.T // 128
        with ExitStack() as es:
            C = self.make_consts(es)
            frB = self.sb(es, "frB", [128, 192], F32)
            self.load(frB, frB.t[:], frq, frq.t.partition_broadcast(128))
            wch = self.sbn(es, "wch", 3, [128, KC, 512], BF16)
            xTt = self.sbn(es, "xTt", 3, [128, KC, 512], BF16)
            acc = self.psn(es, "acc", 4, [128, 512], F32)
            tpp = self.psn(es, "tpp", 2, [128, 512], BF16)
            rp = self.rope_tables(es, "rp")
            tabS = self.sb(es, "tabS", [128, NTS, 192], F32)
            tabO = self.sb(es, "tabO", [128, NTO, 192], F32)
            t1 = self.sb(es, "rt1", [128, 256], F32)
            t2 = self.sb(es, "rt2", [128, 256], F32)
            roped = self.sbn(es, "roped", 2, [128, 512], BF16)
            tstage = self.sbn(es, "tstage", 2, [128, 4, 512], BF16)
            mstage = self.sbn(es, "mstage", 2, [128, 4, 512], BF16)
            fstage = self.sbn(es, "fstage", 3, [128, 512], BF16)
            wsb = self.sb(es, "wsb", [128, NTO, 16], F32)
            wab = self.sb(es, "wab", [128, NTO, 16], F32)
            wsg = self.sb(es, "wsg", [128, NTO, 16], F32)

            for (tabB, possrc, nt) in ((tabS, pos_s, NTS), (tabO, pos_o, NTO)):
                for ti in range(nt):
                    self.compute_rope(rp, frB, possrc, possrc.t[ti * 128:(ti + 1) * 128, :], tabB, tabB.t[:, ti, :])

            def load_w(c0, w):
                b = wch.next()
                self.load(b, b.t[:, :, 0:w], w_in, wv[:, :, c0:c0 + w], q="pool")
                return b

            def load_x(src, ap):
                xb = xTt.next()
                self.load(xb, xb.t[:], src, ap)
                return xb

            def tok_major(xb, sub, wb, w):
                a = acc.next()
                for k in range(KC):
                    self.mm(a, a.t[:, 0:w], xb.t[:, k, sub * 128:(sub + 1) * 128], wb.t[:, k, 0:w],
                            k == 0, k == KC - 1, [xb, wb])
                return a

            def feat_major(xb, wb, c):
                a = acc.next()
                for k in range(KC):
                    self.mm(a, a.t[:], wb.t[:, k, c * 128:(c + 1) * 128], xb.t[:, k, :], k == 0, k == KC - 1, [xb, wb])
                return a

            def transposes(src16, ncol, stage, sub):
                nb = ncol // 128
                tq = tpp.next()
                for c in range(nb):
                    self.tp(tq, tq.t[:, c * 128:(c + 1) * 128], src16.t[:, c * 128:(c + 1) * 128], C["idb"], [src16])
                self.altcopy(stage, stage.t[:, 0:nb, sub * 128:(sub + 1) * 128],
                             tq.t[:, 0:nb * 128].rearrange("p (c t) -> p c t", c=nb), [tq])

            def roped_chunk(xb, wb, st, tabB, dst, h0, nh, hd, cofs, scale, wfold):
                tok = slice(st * 512, (st + 1) * 512)
                stg = tstage.next()
                for sub in range(4):
                    ti = st * 4 + sub
                    a = tok_major(xb, sub, wb, 512)
                    rb = roped.next()
                    self.rope_apply(a, a.t[:], nh, hd, tabB.t[:, ti, cofs:cofs + hd // 2],
                                    tabB.t[:, ti, cofs + hd // 2:cofs + hd], rb, rb.t[:], t1, t2, rd=[tabB])
                    if scale is not None:
                        self.ts(rb, rb.t[:], rb.t[:], scale, None, ALU.mult, None, [rb])
                    if wfold is not None:
                        r3 = rb.t[:].rearrange("p (h d) -> p h d", h=8)
                        self.tt(rb, r3, r3, wab.t[:, ti, wfold:wfold + 8].unsqueeze(2).to_broadcast([128, 8, 64]),
                                ALU.mult, [rb, wab])
                    transposes(rb, 512, stg, sub)
                self.store(dst, dst.t[h0:h0 + 4, :, tok].rearrange("h p t -> p h t"), stg, stg.t[:])

            chunks = []

            def add(c0, w, body):
                chunks.append((c0, w, body))

            for c in range(2):
                def body(wb, c=c):
                    for st in range(self.NST):
                        xb = load_x(xnT_s, xs_ap(st))
                        roped_chunk(xb, wb, st, tabS, sc["kaT"], 4 * c, 4, 128, 0, None, None)
                add(OFF["ka"] + 512 * c, 512, body)
            for (nm, dst) in (("va", sc["va"]), ("vb", sc["vb"])):
                for c in range(2):
                    def body(wb, c=c, dst=dst):
                        for st in range(self.NST):
                            xb = load_x(xnT_s, xs_ap(st))
                            tok = slice(st * 512, (st + 1) * 512)
                            stg = mstage.next()
                            for sub in range(4):
                                a = tok_major(xb, sub, wb, 512)
                                self.altcopy(stg, stg.t[:, sub, :], a.t[:], [a])
                            for hh_ in range(4):
                                self.store(dst, dst.t[4 * c + hh_, :, st * 4:(st + 1) * 4, :],
                                           stg, stg.t[:, :, hh_ * 128:(hh_ + 1) * 128], q="pool")
                    add(OFF[nm] + 512 * c, 512, body)

            def body_ki(wb):
                for st in range(self.NST):
                    xb = load_x(xnT_s, xs_ap(st))
                    tok = slice(st * 512, (st + 1) * 512)
                    stg = tstage.next()
                    for sub in range(4):
                        ti = st * 4 + sub
                        a = tok_major(xb, sub, wb, 64)
                        rb = roped.next()
                        self.rope_apply(a, a.t[:, 0:64], 1, 64, tabS.t[:, ti, 128:160], tabS.t[:, ti, 160:192], rb, rb.t[:, 0:64],
                                        t1, t2, rd=[tabS])
                        tq = tpp.next()
                        self.tp(tq, tq.t[0:64, 0:128], rb.t[:, 0:64], C["idb"], [rb])
                        self.altcopy(stg, stg.t[0:64, 0, sub * 128:(sub + 1) * 128], tq.t[0:64, 0:128], [tq])
                    self.store(sc["kiT"], sc["kiT"].t[:, tok], stg, stg.t[0:64, 0, :], q="pool")
            add(OFF["ki"], 64, body_ki)
            for c2 in range(2):
                def body(wb, c2=c2):
                    for st in range(self.NST):
                        xb = load_x(xnT_s, xs_ap(st))
                        tok = slice(st * 512, (st + 1) * 512)
                        for c in range(4):
                            a = feat_major(xb, wb, c)
                            fs = fstage.next()
                            self.altcopy(fs, fs.t[:], a.t[:], [a])
                            self.store(sc["kbT"], sc["kbT"].t[4 * c2 + c, :, tok], fs, fs.t[:], q="pool")
                add(OFF["kb"] + 512 * c2, 512, body)

            def body_wi(wb):
                for st in range(self.NSLOT):
                    xb = load_x(xnT_o, xo_ap(st))
                    for sub in range(4):
                        a = tok_major(xb, sub, wb, 16)
                        self.copy("dve", wsb, wsb.t[:, st * 4 + sub, :], a.t[:, 0:16], [a])
                self.ts(wab, wab.t[:], wsb.t[:], -1.0, None, ALU.mult, None, [wsb])
                self.tt(wab, wab.t[:], wab.t[:], wsb.t[:], ALU.max, [wab, wsb])
                self.ts(wsg, wsg.t[:], wsb.t[:], 0.0, None, ALU.is_ge, None, [wsb])
                self.ts(wsg, wsg.t[:], wsg.t[:], 2.0, -1.0, ALU.mult, ALU.add, [wsg])
                self.store(sc["wsgn"], sc["wsgn"].t.rearrange("(s p) c -> p s c", p=128), wsg, wsg.t[:], q="pool")
            add(OFF["wi"], 16, body_wi)
            for c in range(2):
                def body(wb, c=c):
                    for st in range(self.NSLOT):
                        xb = load_x(xnT_o, xo_ap(st))
                        roped_chunk(xb, wb, st, tabO, sc["qaT"], 4 * c, 4, 128, 0, SCALE, None)
                add(OFF["qa"] + 512 * c, 512, body)
            for c in range(2):
                def body(wb, c=c):
                    for st in range(self.NSLOT):
                        xb = load_x(xnT_o, xo_ap(st))
                        roped_chunk(xb, wb, st, tabO, sc["qiT"], 4 * c, 8, 64, 128, None, 8 * c)
                add(OFF["qi"] + 512 * c, 512, body)
            for c2 in range(2):
                def body(wb, c2=c2):
                    for st in range(self.NSLOT):
                        xb = load_x(xnT_o, xo_ap(st))
                        tok = slice(st * 512, (st + 1) * 512)
                        for c in range(4):
                            a = feat_major(xb, wb, c)
                            fs = fstage.next()
                            self.actf(fs, fs.t[:], a.t[:], AF.Copy, [a], scale=SCALE)
                            self.store(sc["qbT"], sc["qbT"].t[4 * c2 + c, :, tok], fs, fs.t[:], q="pool")
                add(OFF["qb"] + 512 * c2, 512, body)
            for (nm, dst) in (("ga", sc["gaT"]), ("gb", sc["gbT"])):
                for c2 in range(4):
                    def body(wb, c2=c2, dst=dst):
                        for st in range(self.NSLOT):
                            xb = load_x(xnT_o, xo_ap(st))
                            tok = slice(st * 512, (st + 1) * 512)
                            for c in range(4):
                                a = feat_major(xb, wb, c)
                                fs = fstage.next()
                                self.actf(fs, fs.t[:], a.t[:], AF.Sigmoid, [a])
                                self.store(dst, dst.t[4 * c2 + c, :, tok], fs, fs.t[:], q="pool")
                    add(OFF[nm] + 512 * c2, 512, body)

            wbs = {0: load_w(chunks[0][0], chunks[0][1])}
            for i, (c0, w, body) in enumerate(chunks):
                if i + 1 < len(chunks):
                    wbs[i + 1] = load_w(chunks[i + 1][0], chunks[i + 1][1])
                body(wbs.pop(i))
            self.end_phase()

    def phaseB1(self, sc, dsa_mask):
        P = self.P
        S = self.S
        if self.debug:
            self.dbg_sc = self.dscr(f"dbg_sc_{self.phase_id}", [self.NSLOT * 4, 128, S], F32)
            self.dbg_thr = self.dscr(f"dbg_thr_{self.phase_id}", [self.NSLOT * 4, 128, 1], F32)
            self.dbg_M = self.dscr(f"dbg_M_{self.phase_id}", [self.NSLOT * 4, 128, S], BF16)
            self.dbg_sc2 = self.dscr(f"dbg_sc2_{self.phase_id}", [self.NSLOT * 4, 128, S], F32)
        with ExitStack() as es:
            C = self.make_consts(es)
            if self.debug:
                self.snap = self.sb(es, "snap", [128, S], F32)
            kiT2 = self.sb(es, "kiT2", [128, S], BF16)
            self.load(kiT2, kiT2.t[0:64, :], sc["kiT"], sc["kiT"].t[:, :])
            self.load(kiT2, kiT2.t[64:128, :], sc["kiT"], sc["kiT"].t[:, :])
            NBmax = 4 * (self.NST)
            qiq = self.sbn(es, "qiq", 2, [128, 8, 128], BF16)
            wsg = self.sbn(es, "wsgq", 2, [128, 16], F32)
            dg = self.sbn(es, "dg", 2, [128, 16, 128], BF16)
            Rb = self.sbn(es, "Rb", 4, [128, 512], BF16)
            scb = self.sb(es, "scb", [128, S], F32)
            thr = self.sb(es, "thr", [128, 1], F32)
            mid = self.sb(es, "mid", [128, 1], F32)
            cnt = self.sb(es, "cnt", [128, 1], F32)
            geb = self.sb(es, "geb", [128, 1], F32)
            Mb = self.sb(es, "Mb", [128, S], BF16)
            MT = self.sb(es, "MT", [128, NBmax, 512], BF16)
            mka = self.sbn(es, "mka", 2, [128, 640], F32)
            kaTh = self.sbn(es, "kaTh", 2, [128, S], BF16)
            vah = self.sbn(es, "vah", 2, [128, NBmax, 128], BF16)
            qaTh = self.sbn(es, "qaTh", 2, [128, 512], BF16)
            Pb = self.sbn(es, "Pb", 3, [128, 512], BF16)
            PMb = self.sbn(es, "PMb", 3, [128, 512], BF16)
            rden = self.sb(es, "rden", [128, 512], F32)
            ost = self.sbn(es, "ost", 2, [128, 512], BF16)
            g = self.psn(es, "g", 2, [128, 512], F32)
            scp = self.psn(es, "scp", 2, [128, 512], F32)
            tq = self.psn(es, "tq", 1, [128, 512], BF16)
            Op = self.ps(es, "Op", [128, 512], F32)
            dp = self.ps(es, "dp", [128, 512], F32)

            P.op("pool", lambda e: e.memset(scb.t[:], NEG), writes=[scb])
            for i in range(self.NSLOT):
                smax = 2 * i + 1
                NB = 4 * (smax + 1)
                P.op("pool", lambda e: e.memset(MT.t[:], 0.0), writes=[MT])
                for r in range(4):
                    qt = 4 * i + r
                    nkb = 4 * smax + r + 1
                    n = 128 * nkb
                    tok = slice(qt * 128, (qt + 1) * 128)
                    q = qiq.next()
                    self.load(q, q.t[:], sc["qiT"], sc["qiT"].t[:, :, tok].rearrange("h p t -> p h t"))
                    ws = wsg.next()
                    self.load(ws, ws.t[:], sc["wsgn"], sc["wsgn"].t[tok, :])
                    mk = mka.next()
                    self.load(mk, mk.t[:], dsa_mask, dsa_mask.t[i, r])
                    d = dg.next()
                    for h in range(NIH):
                        self.ts(d, d.t[:, h, :], C["idb"].t[:], ws.t[:, h:h + 1], None, ALU.mult, None, [C["idb"], ws])
                    nch = (n + 511) // 512
                    for c in range(nch):
                        w = min(512, n - 512 * c)
                        ks = slice(512 * c, 512 * c + w)
                        spb = scp.next()
                        pend = None
                        for h in range(NIH):
                            half = (h % 2) * 64
                            lg = g.next()
                            self.mm(lg, lg.t[:, 0:w], q.t[half:half + 64, h // 2, :], kiT2.t[half:half + 64, ks],
                                    True, True, [q, kiT2])
                            Rt = Rb.next()
                            self.actf(Rt, Rt.t[:, 0:w], lg.t[:, 0:w], AF.Relu, [lg])
                            if pend is not None:
                                ph, pR = pend
                                self.mm(spb, spb.t[:, 0:w], d.t[:, ph, :], pR.t[:, 0:w], ph == 0, False, [d, pR])
                            pend = (h, Rt)
                        ph, pR = pend
                        self.mm(spb, spb.t[:, 0:w], d.t[:, ph, :], pR.t[:, 0:w], False, True, [d, pR])
                        self.altcopy(scb, scb.t[:, ks], spb.t[:, 0:w], [spb])
                    self.tt(scb, scb.t[:, n - 640:n], scb.t[:, n - 640:n], mk.t[:], ALU.add, [scb, mk])
                    LO0, W0, NIT = -4096.0, 8192.0, 34
                    if self.debug:
                        self.copy("dve", self.snap, self.snap.t[:, 0:n], scb.t[:, 0:n], [scb])
                        self.store(self.dbg_sc2, self.dbg_sc2.t[qt, :, 0:n], self.snap, self.snap.t[:, 0:n])
                    P.op("dve", lambda e: e.memset(thr.t[:], LO0), writes=[thr])
                    if n > TOPK:
                        for it in range(NIT):
                            hstep = W0 / float(2 ** (it + 1))
                            self.ts(mid, mid.t[:], thr.t[:], hstep, None, ALU.add, None, [thr])
                            P.op("dve", lambda e: e.tensor_scalar(out=Mb.t[:, 0:n], in0=scb.t[:, 0:n], scalar1=mid.t[:, 0:1],
                                                                  scalar2=0.0, op0=ALU.is_ge, op1=ALU.add, accum_out=cnt.t[:, 0:1]),
                                 reads=[scb, mid], writes=[Mb, cnt])
                            self.ts(geb, geb.t[:], cnt.t[:], float(TOPK), None, ALU.is_ge, None, [cnt])
                            self.stt(thr, thr.t[:], geb.t[:], hstep, thr.t[:], ALU.mult, ALU.add, [geb, thr])
                    self.ts(Mb, Mb.t[:, 0:n], scb.t[:, 0:n], thr.t[:, 0:1], None, ALU.is_ge, None, [scb, thr])
                    if self.debug:
                        self.store(self.dbg_sc, self.dbg_sc.t[qt, :, 0:n], scb, scb.t[:, 0:n])
                        self.store(self.dbg_thr, self.dbg_thr.t[qt], thr, thr.t[:])
                        self.store(self.dbg_M, self.dbg_M.t[qt, :, 0:n], Mb, Mb.t[:, 0:n])
                    for j0 in range(0, nkb, 4):
                        nb = min(4, nkb - j0)
                        t = tq.next()
                        for jj in range(nb):
                            j = j0 + jj
                            self.tp(t, t.t[:, jj * 128:(jj + 1) * 128], Mb.t[:, j * 128:(j + 1) * 128], C["idb"], [Mb])
                        self.altcopy(MT, MT.t[:, j0:j0 + nb, r * 128:(r + 1) * 128],
                                     t.t[:, 0:nb * 128].rearrange("p (j t) -> p j t", j=nb), [t])
                tok = slice(i * 512, (i + 1) * 512)
                for h in range(NH):
                    kT = kaTh.next()
                    self.load(kT, kT.t[:, 0:NB * 128], sc["kaT"], sc["kaT"].t[h, :, 0:NB * 128])
                    vh = vah.next()
                    self.load(vh, vh.t[:, 0:NB, :], sc["va"], sc["va"].t[h, :, 0:NB, :])
                    qh = qaTh.next()
                    self.load(qh, qh.t[:], sc["qaT"], sc["qaT"].t[h, :, tok])
                    pend = None
                    for j in range(NB):
                        sT = g.next()
                        self.mm(sT, sT.t[:], kT.t[:, j * 128:(j + 1) * 128], qh.t[:], True, True, [kT, qh])
                        pb = Pb.next()
                        self.actf(pb, pb.t[:], sT.t[:], AF.Exp, [sT])
                        pm = PMb.next()
                        self.tt(pm, pm.t[:], pb.t[:], MT.t[:, j, :], ALU.mult, [pb, MT])
                        if pend is not None:
                            pj, ppm = pend
                            self.mm(Op, Op.t[:], vh.t[:, pj, :], ppm.t[:], pj == 0, False, [vh, ppm])
                            self.mm(dp, dp.t[:], C["ones"].t[:], ppm.t[:], pj == 0, False, [C["ones"], ppm])
                        pend = (j, pm)
                    pj, ppm = pend
                    self.mm(Op, Op.t[:], vh.t[:, pj, :], ppm.t[:], pj == 0, True, [vh, ppm])
                    self.mm(dp, dp.t[:], C["ones"].t[:], ppm.t[:], pj == 0, True, [C["ones"], ppm])
                    P.op("dve", lambda e: e.reciprocal(out=rden.t[:], in_=dp.t[:]), reads=[dp], writes=[rden])
                    o = ost.next()
                    self.tt(o, o.t[:], Op.t[:], rden.t[:], ALU.mult, [Op, rden])
                    self.store(sc["oaT"], sc["oaT"].t[h, :, tok], o, o.t[:], q="pool")
            self.end_phase()

    def phaseB2(self, sc, sb_mask):
        P = self.P
        S = self.S
        with ExitStack() as es:
            C = self.make_consts(es)
            NBmax = 4 * self.NST
            kbTh = self.sbn(es, "kbTh", 2, [128, S], BF16)
            vbh = self.sbn(es, "vbh", 2, [128, NBmax, 128], BF16)
            qbTh = self.sbn(es, "qbTh", 2, [128, 512], BF16)
            msk = self.sbn(es, "sbm", 2, [128, 8, 512], BF16)
            carry = self.sb(es, "carry", [128, 512], F32)
            eb = self.sbn(es, "eb", 2, [128, 512], F32)
            spb = self.sbn(es, "spb", 3, [128, 512], BF16)
            Eb = self.sbn(es, "Eb", 2, [128, 512], F32)
            ab = self.sbn(es, "ab", 3, [128, 512], BF16)
            ost = self.sbn(es, "ostb", 2, [128, 512], BF16)
            zp = self.psn(es, "zp", 3, [128, 512], F32)
            tp_ = self.psn(es, "totp", 2, [128, 512], F32)
            Op = self.ps(es, "Opb", [128, 512], F32)
            for i in range(self.NSLOT):
                smax = 2 * i + 1
                NB = 4 * (smax + 1)
                tok = slice(i * 512, (i + 1) * 512)
                mk = msk.next()
                self.load(mk, mk.t[:], sb_mask, sb_mask.t[i])
                for h in range(NH):
                    kT = kbTh.next()
                    self.load(kT, kT.t[:, 0:NB * 128], sc["kbT"], sc["kbT"].t[h, :, 0:NB * 128])
                    vh = vbh.next()
                    self.load(vh, vh.t[:, 0:NB, :], sc["vb"], sc["vb"].t[h, :, 0:NB, :])
                    qh = qbTh.next()
                    self.load(qh, qh.t[:], sc["qbT"], sc["qbT"].t[h, :, tok])
                    first = True
                    for j in range(NB - 1, -1, -1):
                        jb = j - (NB - 8)
                        z = zp.next()
                        self.mm(z, z.t[:], kT.t[:, j * 128:(j + 1) * 128], qh.t[:], True, True, [kT, qh])
                        e_ = eb.next()
                        self.actf(e_, e_.t[:], z.t[:], AF.Exp, [z])
                        sp = spb.next()
                        self.actf(sp, sp.t[:], e_.t[:], AF.Ln, [e_], bias=1.0)
                        if jb >= 0:
                            self.tt(sp, sp.t[:], sp.t[:], mk.t[:, jb, :], ALU.mult, [sp, mk])
                        self.mm(z, z.t[:], C["negu"].t[:], sp.t[:], False, True, [C["negu"], sp])
                        a = ab.next()
                        if first:
                            self.actf(a, a.t[:], z.t[:], AF.Exp, [z])
                        else:
                            E = Eb.next()
                            self.tt(E, E.t[:], z.t[:], carry.t[:], ALU.subtract, [z, carry])
                            self.actf(a, a.t[:], E.t[:], AF.Exp, [E])
                        if jb >= 0:
                            self.tt(a, a.t[:], a.t[:], mk.t[:, jb, :], ALU.mult, [a, mk])
                        self.mm(Op, Op.t[:], vh.t[:, j, :], a.t[:], first, j == 0, [vh, a])
                        if j > 0:
                            tb = tp_.next()
                            self.mm(tb, tb.t[:], C["ones"].t[:], sp.t[:], True, True, [C["ones"], sp])
                            if first:
                                self.copy("dve", carry, carry.t[:], tb.t[:], [tb])
                            else:
                                self.tt(carry, carry.t[:], carry.t[:], tb.t[:], ALU.add, [carry, tb])
                        first = False
                    o = ost.next()
                    self.copy("act", o, o.t[:], Op.t[:], [Op])
                    self.store(sc["obT"], sc["obT"].t[h, :, tok], o, o.t[:], q="pool")
            self.end_phase()

    def tr16(self, tq, C, src16, dst, dst_sub):
        for k0 in range(0, KC, 8):
            t = tq.next()
            for k in range(8):
                self.tp(t, t.t[:, k * 128:(k + 1) * 128], src16.t[:, (k0 + k) * 128:(k0 + k + 1) * 128], C["idb"], [src16])
            self.altcopy(dst, dst.t[:, k0:k0 + 8, dst_sub], t.t[:].rearrange("p (k t) -> p k t", k=8), [t])

    def phaseC1(self, W, sc, xres, xmres, xmT_d):
        P = self.P
        with ExitStack() as es:
            C = self.make_consts(es)
            gM = self.bcast_load(es, "gM", W["lnm_g"], W["lnm_g"].t)
            bM = self.bcast_load(es, "bM", W["lnm_b"], W["lnm_b"].t)
            tmp = self.ln_tmp(es, "pc")
            w4 = self.sbn(es, "w4", 4, [128, NH, 256], BF16)
            wd = self.sbn(es, "wo2", 2, [128, 2, D], BF16)
            oT = self.sbn(es, "oT", 2, [128, NH, 512], BF16)
            gT = self.sbn(es, "gT", 2, [128, 4, 512], BF16)
            m1 = self.sb(es, "m1", [128, 512], F32)
            mT = self.sb(es, "mT", [128, KC, 512], BF16)
            accb = self.sb(es, "accb", [128, 4, D], F32)
            x16 = self.sbn(es, "x16", 2, [128, D], BF16)
            xmT = self.sb(es, "xmT", [128, KC, 512], BF16)
            bigs = [self.ps(es, f"bigc{c}", [128, 512], F32) for c in range(4)]
            pq = self.psn(es, "pq", 3, [128, 512], F32)
            tq = self.psn(es, "tq", 1, [128, 1024], BF16)
            for ti in range(self.NSLOT):
                tok = slice(ti * 512, (ti + 1) * 512)
                oa = oT.next()
                self.load(oa, oa.t[:], sc["oaT"], sc["oaT"].t[:, :, tok].rearrange("h p t -> p h t"))
                ob = oT.next()
                self.load(ob, ob.t[:], sc["obT"], sc["obT"].t[:, :, tok].rearrange("h p t -> p h t"))
                for sub in range(4):
                    self.load(accb, accb.t[:, sub, :], xres, xres.t[ti * 512 + sub * 128: ti * 512 + (sub + 1) * 128, :])
                for fg in range(4):
                    ga = gT.next()
                    self.load(ga, ga.t[:], sc["gaT"], sc["gaT"].t[4 * fg:4 * fg + 4, :, tok].rearrange("c p t -> p c t"))
                    gb = gT.next()
                    self.load(gb, gb.t[:], sc["gbT"], sc["gbT"].t[4 * fg:4 * fg + 4, :, tok].rearrange("c p t -> p c t"))
                    for half in range(2):
                        c0 = fg * 512 + half * 256
                        wa = w4.next()
                        self.load(wa, wa.t[:], W["pa"], W["pa"].t[:, c0:c0 + 256].rearrange("(h p) c -> p h c", p=128), q="pool")
                        wb_ = w4.next()
                        self.load(wb_, wb_.t[:], W["pb"], W["pb"].t[:, c0:c0 + 256].rearrange("(h p) c -> p h c", p=128), q="pool")
                        for c in range(2):
                            fc = fg * 4 + half * 2 + c
                            gi = half * 2 + c
                            pa = pq.next()
                            for h in range(NH):
                                self.mm(pa, pa.t[:], wa.t[:, h, c * 128:(c + 1) * 128], oa.t[:, h, :], h == 0, h == NH - 1, [wa, oa])
                            pb = pq.next()
                            for h in range(NH):
                                self.mm(pb, pb.t[:], wb_.t[:, h, c * 128:(c + 1) * 128], ob.t[:, h, :], h == 0, h == NH - 1, [wb_, ob])
                            self.tt(m1, m1.t[:], pa.t[:], ga.t[:, gi, :], ALU.mult, [pa, ga])
                            self.tt(mT, mT.t[:, fc, :], pb.t[:], gb.t[:, gi, :], ALU.mult, [pb, gb])
                            self.tt(mT, mT.t[:, fc, :], mT.t[:, fc, :], m1.t[:], ALU.add, [mT, m1])
                for sub in range(4):
                    self.actf(accb, accb.t[:, sub, :], accb.t[:, sub, :], AF.Copy, [accb], scale=ALPHA)
                for kg in range(8):
                    wo = wd.next()
                    self.load(wo, wo.t[:], W["wo"], W["wo"].t[kg * 256:(kg + 1) * 256, :].rearrange("(k p) c -> p k c", p=128), q="pool")
                    for sub in range(4):
                        for c in range(4):
                            bg = bigs[c]
                            cs = slice(c * 512, (c + 1) * 512)
                            for k in range(2):
                                self.mm(bg, bg.t[:], mT.t[:, kg * 2 + k, sub * 128:(sub + 1) * 128],
                                        wo.t[:, k, cs], k == 0, k == 1, [mT, wo])
                            self.tt(accb, accb.t[:, sub, cs], accb.t[:, sub, cs], bg.t[:], ALU.add, [accb, bg])
                for sub in range(4):
                    b16 = x16.next()
                    self.layer_norm(accb, accb.t[:, sub, :], gM, bM, accb, accb.t[:, sub, :], b16, b16.t[:], tmp)
                    self.store(xmres, xmres.t[ti * 512 + sub * 128: ti * 512 + (sub + 1) * 128, :], accb, accb.t[:, sub, :])
                    self.tr16(tq, C, b16, xmT, slice(sub * 128, (sub + 1) * 128))
                self.store(xmT_d, xmT_d.t[ti], xmT, xmT.t[:])
            self.end_phase()

    def phaseC2(self, W, xmres, xmT_d, facc, moe):
        P = self.P
        dff = D_EXP if moe else D_FF
        NF = dff // 128
        nexp = NE if moe else 1
        PIECE = 4
        TT = min(1024, self.T)
        NH2 = TT // 512
        NSUB = TT // 128
        with ExitStack() as es:
            C = self.make_consts(es)
            w8 = self.sbn(es, "w8", 3, [128, KC, 256], BF16)
            wd = self.sbn(es, "wd2", 2, [128, 4, D], BF16)
            accb = self.sb(es, "accb2", [128, NSUB, D], F32)
            xT = self.sb(es, "xmT2", [128, KC, TT], BF16)
            actT = self.sbn(es, "actT", 2, [128, PIECE, TT], BF16)
            sg = self.sbn(es, "sg", 2, [128, 512], F32)
            x16 = self.sb(es, "x16b", [128, D], BF16)
            bigs = [self.ps(es, f"big{c}", [128, 512], F32) for c in range(4)]
            pq = self.psn(es, "pq2", 3, [128, 512], F32)
            tq = self.psn(es, "tq2", 1, [128, 1024], BF16)
            if moe:
                xlf = self.sb(es, "xlf", [128, D], F32)
                if TT >= 1024:
                    a0, a1 = actT.bufs[0], actT.bufs[1]
                    xhT = _View(a0, a0.t[:, 0:2, :].rearrange("p a (b t) -> p (a b) t", t=128))
                    xlT = _View(a0, a0.t[:, 2:4, :].rearrange("p a (b t) -> p (a b) t", t=128))
                    xlo = _View(a1, a1.t[:, 0:2, :].rearrange("p a t -> p (a t)"))
                else:
                    b1_, b2_, b3_ = (self.sb(es, "xhTs", [128, KC, 128], BF16), self.sb(es, "xlTs", [128, KC, 128], BF16),
                                     self.sb(es, "xlos", [128, D], BF16))
                    xhT, xlT, xlo = _View(b1_, b1_.t[:]), _View(b2_, b2_.t[:]), _View(b3_, b3_.t[:])
                rwf = self.sb(es, "rwf", [128, KC, NE], F32)
                rwh = self.sb(es, "rwh", [128, KC, NE], BF16)
                rwl = self.sb(es, "rwl", [128, KC, NE], BF16)
                rwt = self.sb(es, "rwt", [128, KC, NE], F32)
                self.load(rwf, rwf.t[:], W["rw"], W["rw"].t.rearrange("(k p) e -> p k e", p=128))
                self.copy("dve", rwh, rwh.t[:], rwf.t[:], [rwf])
                self.copy("dve", rwt, rwt.t[:], rwh.t[:], [rwh])
                self.tt(rwt, rwt.t[:], rwf.t[:], rwt.t[:], ALU.subtract, [rwf, rwt])
                self.copy("dve", rwl, rwl.t[:], rwt.t[:], [rwt])
                lgs = self.sb(es, "lgs", [128, NE], F32)
                l8 = self.sb(es, "l8", [128, 8], F32)
                gts = self.sb(es, "gts", [128, 4], F32)
                eq = self.sb(es, "eq", [128, 2, NE], F32)
                comb = self.sb(es, "comb", [128, NSUB, NE], F32)
            for ti in range(self.T // TT):
                for hh in range(NH2):
                    self.load(xT, xT.t[:, :, hh * 512:(hh + 1) * 512], xmT_d, xmT_d.t[ti * NH2 + hh])
                for sub in range(NSUB):
                    rows = slice(ti * TT + sub * 128, ti * TT + (sub + 1) * 128)
                    self.load(accb, accb.t[:, sub, :], xmres, xmres.t[rows, :])
                    if moe:
                        self.copy("act", x16, x16.t[:], accb.t[:, sub, :], [accb])
                        self.copy("dve", xlf, xlf.t[:], x16.t[:], [x16])
                        self.tt(xlo, xlo.t, accb.t[:, sub, :], xlf.t[:], ALU.subtract, [accb, xlf])
                        for (srcb, sap, dstT) in ((x16, x16.t, xhT), (xlo, xlo.t, xlT)):
                            for k0 in range(0, KC, 8):
                                t = tq.next()
                                for k in range(8):
                                    self.tp(t, t.t[:, k * 128:(k + 1) * 128], sap[:, (k0 + k) * 128:(k0 + k + 1) * 128], C["idb"], [srcb])
                                self.altcopy(dstT, dstT.t[:, k0:k0 + 8, :], t.t[:].rearrange("p (k t) -> p k t", k=8), [t])
                        lp = pq.next()
                        n3 = 3 * KC
                        idx = 0
                        for (a_, b_) in ((xhT, rwh), (xlT, rwh), (xhT, rwl)):
                            for k in range(KC):
                                self.mm(lp, lp.t[:, 0:NE], a_.t[:, k, :], b_.t[:, k, :], idx == 0, idx == n3 - 1, [a_, b_])
                                idx += 1
                        self.copy("dve", lgs, lgs.t[:], lp.t[:, 0:NE], [lp])
                        P.op("dve", lambda e: e.max(out=l8.t[:], in_=lgs.t[:]), reads=[lgs], writes=[l8])
                        self.tt(gts, gts.t[:, 0:1], l8.t[:, 0:1], l8.t[:, 1:2], ALU.subtract, [l8])
                        self.actf(gts, gts.t[:, 1:2], gts.t[:, 0:1], AF.Sigmoid, [gts])
                        self.ts(gts, gts.t[:, 2:3], gts.t[:, 1:2], -1.0, 1.0, ALU.mult, ALU.add, [gts])
                        self.ts(eq, eq.t[:, 0, :], lgs.t[:], l8.t[:, 0:1], None, ALU.is_equal, None, [lgs, l8])
                        self.ts(eq, eq.t[:, 0, :], eq.t[:, 0, :], gts.t[:, 1:2], None, ALU.mult, None, [eq, gts])
                        self.ts(eq, eq.t[:, 1, :], lgs.t[:], l8.t[:, 1:2], None, ALU.is_equal, None, [lgs, l8])
                        self.ts(eq, eq.t[:, 1, :], eq.t[:, 1, :], gts.t[:, 2:3], None, ALU.mult, None, [eq, gts])
                        self.tt(comb, comb.t[:, sub, :], eq.t[:, 0, :], eq.t[:, 1, :], ALU.add, [eq])
                    self.actf(accb, accb.t[:, sub, :], accb.t[:, sub, :], AF.Copy, [accb], scale=ALPHA)
                for ex in range(nexp):
                    if moe:
                        Wg, Wu, Wdn = W["mg"].t[ex], W["mu"].t[ex], W["md"].t[ex]
                        wgB, wuB, wdB = W["mg"], W["mu"], W["md"]
                    else:
                        Wg, Wu, Wdn = W["wg"].t, W["wu"].t, W["wd"].t
                        wgB, wuB, wdB = W["wg"], W["wu"], W["wd"]
                    Wgv = Wg.rearrange("(k p) c -> p k c", p=128)
                    Wuv = Wu.rearrange("(k p) c -> p k c", p=128)
                    for p0 in range(0, NF, PIECE):
                        aT = actT.next()
                        for f2 in range(PIECE // 2):
                            c0 = (p0 + 2 * f2) * 128
                            wgc = w8.next()
                            self.load(wgc, wgc.t[:], wgB, Wgv[:, :, c0:c0 + 256], q="pool")
                            wuc = w8.next()
                            self.load(wuc, wuc.t[:], wuB, Wuv[:, :, c0:c0 + 256], q="pool")
                            for c in range(2):
                                for hh in range(NH2):
                                    ts_ = slice(hh * 512, (hh + 1) * 512)
                                    gp = pq.next()
                                    for k in range(KC):
                                        self.mm(gp, gp.t[:], wgc.t[:, k, c * 128:(c + 1) * 128], xT.t[:, k, ts_], k == 0, k == KC - 1, [wgc, xT])
                                    up = pq.next()
                                    for k in range(KC):
                                        self.mm(up, up.t[:], wuc.t[:, k, c * 128:(c + 1) * 128], xT.t[:, k, ts_], k == 0, k == KC - 1, [wuc, xT])
                                    s_ = sg.next()
                                    self.actf(s_, s_.t[:], gp.t[:], AF.Silu, [gp])
                                    self.tt(aT, aT.t[:, 2 * f2 + c, ts_], s_.t[:], up.t[:], ALU.mult, [s_, up])
                        wdc = wd.next()
                        r0 = p0 * 128
                        self.load(wdc, wdc.t[:], wdB, Wdn[r0:r0 + 512, :].rearrange("(k p) c -> p k c", p=128), q="pool")
                        for sub in range(NSUB):
                            for c in range(4):
                                bg = bigs[c]
                                for k in range(PIECE):
                                    self.mm(bg, bg.t[:], aT.t[:, k, sub * 128:(sub + 1) * 128],
                                            wdc.t[:, k, c * 512:(c + 1) * 512], k == 0, k == PIECE - 1, [aT, wdc])
                                cs = slice(c * 512, (c + 1) * 512)
                                if moe:
                                    self.stt(accb, accb.t[:, sub, cs], bg.t[:], comb.t[:, sub, ex:ex + 1], accb.t[:, sub, cs],
                                             ALU.mult, ALU.add, [bg, comb, accb])
                                else:
                                    self.tt(accb, accb.t[:, sub, cs], accb.t[:, sub, cs], bg.t[:], ALU.add, [accb, bg])
                for sub in range(NSUB):
                    rows = slice(ti * TT + sub * 128, ti * TT + (sub + 1) * 128)
                    self.store(facc, facc.t[rows, :], accb, accb.t[:, sub, :])
            self.end_phase()

    def phaseC3(self, W, facc, out_res, out_T):
        with ExitStack() as es:
            C = self.make_consts(es)
            gF = self.bcast_load(es, "gF", W["lnf_g"], W["lnf_g"].t)
            bF = self.bcast_load(es, "bF", W["lnf_b"], W["lnf_b"].t)
            tmp = self.ln_tmp(es, "pc3")
            xin = self.sbn(es, "c3x", 3, [128, D], F32)
            x16 = self.sbn(es, "c3x16", 2, [128, D], BF16)
            xT = self.sbn(es, "c3xT", 2, [128, KC, 512], BF16)
            tq = self.psn(es, "tq3", 2, [128, 1024], BF16)
            for ti in range(self.T // 128):
                rows = slice(ti * 128, (ti + 1) * 128)
                xi = xin.next()
                self.load(xi, xi.t[:], facc, facc.t[rows, :])
                sub = ti % 4
                if out_T is None:
                    self.layer_norm(xi, xi.t[:], gF, bF, xi, xi.t[:], None, None, tmp)
                else:
                    b16 = x16.next()
                    self.layer_norm(xi, xi.t[:], gF, bF, xi, xi.t[:], b16, b16.t[:], tmp)
                    if sub == 0:
                        xTb = xT.next()
                    self.tr16(tq, C, b16, xTb, slice(sub * 128, (sub + 1) * 128))
                    if sub == 3:
                        self.store(out_T, out_T.t[ti // 4], xTb, xTb.t[:], q="pool")
                self.store(out_res, out_res.t[rows, :], xi, xi.t[:], q="pool")
            self.end_phase()

    def scratch(self, L):
        S, T = self.S, self.T
        p = f"L{L}_"
        return {
            "kaT": self.dscr(p + "kaT", [NH, 128, S], BF16), "kbT": self.dscr(p + "kbT", [NH, 128, S], BF16),
            "kiT": self.dscr(p + "kiT", [DI, S], BF16),
            "va": self.dscr(p + "va", [NH, 128, S // 128, DH], BF16), "vb": self.dscr(p + "vb", [NH, 128, S // 128, DH], BF16),
            "qaT": self.dscr(p + "qaT", [NH, 128, T], BF16), "qbT": self.dscr(p + "qbT", [NH, 128, T], BF16),
            "qiT": self.dscr(p + "qiT", [8, 128, T], BF16), "wsgn": self.dscr(p + "wsgn", [T, NIH], F32),
            "gaT": self.dscr(p + "gaT", [KC, 128, T], BF16), "gbT": self.dscr(p + "gbT", [KC, 128, T], BF16),
            "oaT": self.dscr(p + "oaT", [NH, 128, T], BF16), "obT": self.dscr(p + "obT", [NH, 128, T], BF16),
        }

    def layer_weights(self, L):
        p = f"l{L}_"
        W = {"w_in": self.din(p + "w_in", [D, IN_COLS], F32), "pa": self.din(p + "pa", [NH * DH, D], F32),
             "pb": self.din(p + "pb", [NH * DH, D], F32), "wo": self.din(p + "wo", [D, D], F32),
             "lnm_g": self.din(p + "lnm_g", [D], F32), "lnm_b": self.din(p + "lnm_b", [D], F32),
             "lnf_g": self.din(p + "lnf_g", [D], F32), "lnf_b": self.din(p + "lnf_b", [D], F32)}
        if L % 2 == 0:
            W.update(wg=self.din(p + "wg", [D, D_FF], F32), wu=self.din(p + "wu", [D, D_FF], F32),
                     wd=self.din(p + "wd", [D_FF, D], F32))
        else:
            W.update(rw=self.din(p + "rw", [D, NE], F32), mg=self.din(p + "mg", [NE, D, D_EXP], F32),
                     mu=self.din(p + "mu", [NE, D, D_EXP], F32), md=self.din(p + "md", [NE, D_EXP, D], F32))
        return W

    def build(self):
        S, T = self.S, self.T
        nc = self.nc
        with self.ges:
            xs = self.din("xs", [S, D], F32)
            xo = self.din("xo", [T, D], F32)
            pos_s = self.din("pos_s", [S, 1], I32)
            pos_o = self.din("pos_o", [T, 1], I32)
            frq = self.din("frq", [192], F32)
            dsa_mask = self.din("dsa_mask", [self.NSLOT, 4, 128, 640], F32)
            sb_mask = self.din("sb_mask", [self.NSLOT, 128, 8, 512], BF16)
            y = Buf(nc.dram_tensor("y", [T, D], F32, kind="ExternalOutput").ap())
            xnT_s = self.dscr("xnT_s", [self.NST, 128, KC, 512], BF16)
            xnT_o = self.dscr("xnT_o", [self.NSLOT, 128, KC, 512], BF16)
            xres = self.dscr("xres", [T, D], F32)
            first = self.layers[0]
            if first == 0:
                lng = self.din("ln_in_g", [D], F32)
                lnb = self.din("ln_in_b", [D], F32)
                self.phase0(xs, xo, True, lng, lnb, xnT_s, xnT_o, xres)
            else:
                self.phase0(xs, xo, False, None, None, xnT_s, xnT_o, xres)
            xs_src, xs_ap = xnT_s, (lambda st: xnT_s.t[st])
            xo_src, xo_ap = xnT_o, (lambda i: xnT_o.t[i])
            nl = len(self.layers)
            for li, L in enumerate(self.layers):
                last = (li == nl - 1)
                W = self.layer_weights(L)
                sc = self.scratch(L)
                self.phaseA(L, W["w_in"], xs_src, xs_ap, xo_src, xo_ap, pos_s, pos_o, frq, sc)
                self.phaseB1(sc, dsa_mask)
                self.phaseB2(sc, sb_mask)
                xmres = self.dscr(f"L{L}_xmres", [T, D], F32)
                xmT_d = self.dscr(f"L{L}_xmT", [self.NSLOT, 128, KC, 512], BF16)
                self.phaseC1(W, sc, xres, xmres, xmT_d)
                facc = self.dscr(f"L{L}_facc", [T, D], F32)
                self.phaseC2(W, xmres, xmT_d, facc, moe=(L % 2 == 1))
                if last:
                    self.phaseC3(W, facc, y, None)
                else:
                    NSL = self.NSLOT
                    ag_src = Buf(nc.dram_tensor(f"ag_src{L}", [NSL, 128, KC * 512], BF16, kind="Internal").ap())
                    ag_dst = Buf(nc.dram_tensor(f"ag_dst{L}", [NSL, 256, KC * 512], BF16, kind="Internal").ap())
                    src_v = ag_src.t.rearrange("s p (k t) -> s p k t", k=KC)
                    self.phaseC3(W, facc, xres, _View(ag_src, src_v))
                    groups = [[2 * b, 2 * b + 1] for b in range(self.ncores // 2)]
                    for i in range(NSL):
                        self.P.coll((lambda i: lambda e: e.collective_compute(
                            "AllGather", ALU.bypass, replica_groups=groups,
                            ins=[ag_src.t[i].opt()], outs=[ag_dst.t[i].opt()]))(i),
                            reads=[ag_src], writes=[ag_dst])
                    self.end_phase()
                    where = {}
                    for p in range(2):
                        for i, st in enumerate(own_tiles(self.NST, p)):
                            where[st] = (i, p)
                    dst_v = ag_dst.t.rearrange("s (r p) (k t) -> s r p k t", r=2, k=KC)
                    xs_src, xs_ap = ag_dst, (lambda st, dst_v=dst_v, where=where: dst_v[where[st][0], where[st][1]])
                    xo_src, xo_ap = ag_src, (lambda i, src_v=src_v: src_v[i])
        return nc


def own_tiles(NST, p):
    out = []
    for i in range(NST // 2):
        a, b = 2 * i, 2 * i + 1
        if i % 2 == 1:
            a, b = b, a
        out.append(a if p == 0 else b)
    return out


def host_consts(S, p):
    NST = S // 512
    own = own_tiles(NST, p)
    nsl = len(own)
    dsa = np.zeros((nsl, 4, 128, 640), np.float32)
    sbm = np.zeros((nsl, 128, 8, 512), np.float32)
    for i, st in enumerate(own):
        smax = 2 * i + 1
        NB = 4 * (smax + 1)
        for r in range(4):
            n = 128 * (4 * smax + r + 1)
            s = (n - 640) + np.arange(640)
            t = 512 * st + 128 * r + np.arange(128)
            adm = (s[None, :] // 64) <= (t[:, None] // 64)
            dsa[i, r] = np.where(adm, 0.0, NEG)
        t = 512 * st + np.arange(512)
        for jb in range(8):
            s = 128 * (NB - 8 + jb) + np.arange(128)
            sbm[i, :, jb, :] = (s[:, None] < t[None, :]).astype(np.float32)
    frh = (10000.0 ** (-np.arange(0, DH, 2, dtype=np.float32) / DH)).astype(np.float32)
    fri = (10000.0 ** (-np.arange(0, DI, 2, dtype=np.float32) / DI)).astype(np.float32)
    frq = np.concatenate([frh, frh, fri, fri]).astype(np.float32)
    return dsa, sbm.astype(ml_dtypes.bfloat16), frq, own


_NC_CACHE = {}


def get_nc(S, layers, debug=False, ncores=8):
    key = (S, tuple(layers), debug, ncores)
    if key not in _NC_CACHE:
        _NC_CACHE[key] = Builder(S, list(layers), debug, ncores).build()
    return _NC_CACHE[key]


def layer_inputs(inp, L):
    p = f"l{L}_"
    m = {p + "w_in": inp["w_in"][L], p + "pa": inp["w_proj_a"][L], p + "pb": inp["w_proj_b"][L],
         p + "wo": inp["w_out"][L], p + "lnm_g": inp["ln_mix_g"][L], p + "lnm_b": inp["ln_mix_b"][L],
         p + "lnf_g": inp["ln_ffn_g"][L], p + "lnf_b": inp["ln_ffn_b"][L]}
    i = L // 2
    if L % 2 == 0:
        m.update({p + "wg": inp["ffn_w_gate"][i], p + "wu": inp["ffn_w_up"][i], p + "wd": inp["ffn_w_down"][i]})
    else:
        m.update({p + "rw": inp["router_w"][i], p + "mg": inp["moe_w_gate"][i], p + "mu": inp["moe_w_up"][i],
                  p + "md": inp["moe_w_down"][i]})
    return {k: np.ascontiguousarray(v) for k, v in m.items()}


def run_layers(xfull, inp, layers, debug=False):
    B, S, _ = xfull.shape
    nc = get_nc(S, layers, debug, 2 * B)
    pos = np.asarray(inp["positions"]).astype(np.int32)
    in_maps = []
    owns = []
    wmap = {}
    for L in layers:
        wmap.update(layer_inputs(inp, L))
    if layers[0] == 0:
        wmap["ln_in_g"] = np.ascontiguousarray(inp["ln_in_g"])
        wmap["ln_in_b"] = np.ascontiguousarray(inp["ln_in_b"])
    for c in range(2 * B):
        b, p = c // 2, c % 2
        dsa, sbm, frq, own = host_consts(S, p)
        rows = np.concatenate([np.arange(st * 512, (st + 1) * 512) for st in own])
        owns.append(rows)
        m = {"xs": np.ascontiguousarray(xfull[b]), "xo": np.ascontiguousarray(xfull[b][rows]),
             "pos_s": np.ascontiguousarray(pos[b].reshape(S, 1)), "pos_o": np.ascontiguousarray(pos[b][rows].reshape(-1, 1)),
             "frq": frq, "dsa_mask": dsa, "sb_mask": sbm}
        m.update(wmap)
        in_maps.append(m)
    res = run_bass_kernel_spmd(nc, in_maps, core_ids=list(range(2 * B)))
    out = np.zeros((B, S, D), np.float32)
    for c in range(2 * B):
        out[c // 2][owns[c]] = res.results[c]["y"]
    return out, res


def kernel(**inputs):
    inp = {k: np.asarray(v) for k, v in inputs.items()}
    x = inp["x"].astype(np.float32)
    x2, _ = run_layers(x, inp, [0, 1])
    return x2
```

```python
import math
from contextlib import ExitStack

import numpy as np
import ml_dtypes
import concourse.bass as bass
import concourse.mybir as mybir
from concourse.bass_utils import run_bass_kernel_spmd

F32 = mybir.dt.float32
BF16 = mybir.dt.bfloat16
I32 = mybir.dt.int32
ALU = mybir.AluOpType
AF = mybir.ActivationFunctionType

D = 2048
KC = 16
NH = 8
DH = 128
NIH = 16
DI = 64
IN_COLS = 11344
OFF = dict(qa=0, ka=1024, va=2048, qi=3072, ki=4096, wi=4160, qb=4176, kb=5200, vb=6224, ga=7248, gb=9296)
D_FF = 5632
D_EXP = 7168
NE = 8
TOPK = 256
LN_EPS = 1e-5
ALPHA = (2.0 * 2) ** 0.25
SCALE = 1.0 / math.sqrt(DH)
NEG = -1.0e30
NDSEM = 6


class Trk:
    __slots__ = ("w", "r")

    def __init__(self):
        self.w = None
        self.r = {}


class Buf:
    __slots__ = ("t", "k")

    def __init__(self, t):
        self.t = t
        self.k = Trk()


class Prog:
    ENGS = ("pe", "act", "dve", "pool", "sp")

    def __init__(self, nc, es):
        self.nc = nc
        self.es = es
        self.ops = {e: [] for e in self.ENGS}
        self.cnt = {}
        self.known = {e: {} for e in self.ENGS}
        self.dma_i = {e: 0 for e in self.ENGS}
        self.sems = {}
        self.nops = 0

    def _sem(self, k):
        s = self.sems.get(k)
        if s is None:
            nm = k if isinstance(k, str) else f"d_{k[1]}_{k[2]}"
            s = self.es.enter_context(self.nc.semaphore("s_" + nm))
            self.sems[k] = s
        return s

    def _collect(self, eng, reads, writes):
        waits = {}

        def need(kv):
            if kv is None:
                return
            k, v = kv
            if k == eng and eng == "pe":
                return
            if waits.get(k, 0) < v:
                waits[k] = v

        for t in reads:
            need(t.w)
        for t in writes:
            need(t.w)
            for kv in t.r.items():
                need(kv)
        kn = self.known[eng]
        out = []
        for k, v in waits.items():
            if kn.get(k, 0) >= v:
                continue
            kn[k] = v
            out.append((k, v))
        return out

    def op(self, eng, fn, reads=(), writes=()):
        reads = [b.k for b in reads]
        writes = [b.k for b in writes]
        waits = self._collect(eng, reads, writes)
        v = self.cnt.get(eng, 0) + 1
        self.cnt[eng] = v
        self.ops[eng].append((waits, fn, (eng, 1), v))
        self.nops += 1
        for t in reads:
            if t.r.get(eng, 0) < v:
                t.r[eng] = v
        for t in writes:
            t.w = (eng, v)
            t.r = {}

    def dma(self, q, fn, reads=(), writes=()):
        reads = [b.k for b in reads]
        writes = [b.k for b in writes]
        i = self.dma_i[q]
        self.dma_i[q] = i + 1
        key = ("d", q, i % NDSEM)
        waits = self._collect(q, reads, writes)
        prev = 16 * (i // NDSEM)
        if prev > 0 and self.known[q].get(key, 0) < prev:
            self.known[q][key] = prev
            waits.append((key, prev))
        v = prev + 16
        self.cnt[key] = v
        self.ops[q].append((waits, fn, (key, 16), v))
        self.nops += 1
        for t in reads:
            if t.r.get(key, 0) < v:
                t.r[key] = v
        for t in writes:
            t.w = (key, v)
            t.r = {}

    def coll(self, fn, reads=(), writes=()):
        reads = [b.k for b in reads]
        writes = [b.k for b in writes]
        key = ("d", "cc", self.dma_i.get("cc", 0))
        self.dma_i["cc"] = self.dma_i.get("cc", 0) + 1
        waits = self._collect("pool", reads, writes)
        self.cnt[key] = 1
        self.ops["pool"].append((waits, fn, (key, 1), 1))
        self.nops += 1
        for t in reads:
            t.r[key] = 1
        for t in writes:
            t.w = (key, 1)
            t.r = {}

    def barrier(self):
        snap = dict(self.cnt)
        for e in self.ENGS:
            waits = []
            for k, v in snap.items():
                if k == e and e == "pe":
                    continue
                if self.known[e].get(k, 0) < v:
                    self.known[e][k] = v
                    waits.append((k, v))
            if waits:
                self.ops[e].append((waits, None, None, 0))

    def emit(self):
        nc = self.nc
        if not hasattr(self, "base_new"):
            self.base_new = {}
            self.base_old = {}
        targets = {e: set() for e in self.ENGS}
        for e in self.ENGS:
            for waits, fn, inc, v in self.ops[e]:
                for k, wv in waits:
                    self._sem(k)
                    if isinstance(k, str):
                        targets[k].add(wv)
                if inc is not None:
                    self._sem(inc[0])
        remap = {}
        for e in self.ENGS:
            newc = self.base_new.get(e, 0)
            m = {}
            for wv in targets[e]:
                if wv <= self.base_old.get(e, 0):
                    m[wv] = self.base_new.get(e, 0)
            for waits, fn, inc, v in self.ops[e]:
                if fn is None or inc is None or not isinstance(inc[0], str):
                    continue
                if v in targets[e]:
                    newc += 1
                    m[v] = newc
            remap[e] = m
            self.base_new[e] = newc
            self.base_old[e] = self.cnt.get(e, 0)
        sems = self.sems
        with nc.Block() as block:
            engmap = {"pe": block.tensor, "act": block.scalar, "dve": block.vector,
                      "pool": block.gpsimd, "sp": block.sync}

            def mk(e, oplist):
                tg = targets[e]

                def body(eng):
                    for waits, fn, inc, v in oplist:
                        for k, wv in waits:
                            eng.wait_ge(sems[k], remap[k][wv] if isinstance(k, str) else wv)
                        if fn is not None:
                            ins = fn(eng)
                            if isinstance(inc[0], str):
                                if v in tg:
                                    ins.then_inc(sems[inc[0]], 1)
                            else:
                                ins.then_inc(sems[inc[0]], inc[1])
                return body

            for e in self.ENGS:
                if self.ops[e]:
                    engmap[e](mk(e, self.ops[e]))
        self.ops = {e: [] for e in self.ENGS}


class _View:
    __slots__ = ("t", "k")

    def __init__(self, base, ap):
        self.t = ap
        self.k = base.k


class Rot:
    def __init__(self, bufs):
        self.bufs = bufs
        self.i = 0

    def next(self):
        b = self.bufs[self.i % len(self.bufs)]
        self.i += 1
        return b


class Builder:
    def __init__(self, S, layers, debug=False, ncores=8):
        self.ncores = ncores
        self.S = S
        self.T = S // 2
        self.NST = S // 512
        self.NSLOT = self.NST // 2
        self.layers = layers
        self.debug = debug
        self.nc = bass.Bass("TRN2", target_bir_lowering=False)
        self.ges = ExitStack()
        self.P = Prog(self.nc, self.ges)
        self.alt = 0
        self.phase_id = 0

    def din(self, name, shape, dt):
        return Buf(self.nc.dram_tensor(name, list(shape), dt, kind="ExternalInput").ap())

    def dscr(self, name, shape, dt):
        kind = "ExternalOutput" if self.debug else "Internal"
        return Buf(self.nc.dram_tensor(name, list(shape), dt, kind=kind).ap())

    def sb(self, es, name, shape, dt):
        return Buf(es.enter_context(self.nc.sbuf_tensor(f"p{self.phase_id}_{name}", list(shape), dt)))

    def ps(self, es, name, shape, dt):
        return Buf(es.enter_context(self.nc.psum_tensor(f"p{self.phase_id}_{name}", list(shape), dt)))

    def sbn(self, es, name, n, shape, dt):
        return Rot([self.sb(es, f"{name}{i}", shape, dt) for i in range(n)])

    def psn(self, es, name, n, shape, dt):
        return Rot([self.ps(es, f"{name}{i}", shape, dt) for i in range(n)])

    def mm(self, ob, oap, lhsT, rhs, start, stop, rd):
        self.P.op("pe", lambda e: e.matmul(oap, lhsT, rhs, start=start, stop=stop, skip_group_check=True),
                  reads=rd, writes=[ob])

    def tp(self, ob, oap, in_ap, ident, rd):
        self.P.op("pe", lambda e: e.transpose(oap, in_ap, ident.t[:]), reads=rd + [ident], writes=[ob])

    def actf(self, ob, oap, iap, func, rd, bias=None, scale=None):
        kw = {}
        if bias is not None:
            kw["bias"] = bias
        if scale is not None:
            kw["scale"] = scale
        self.P.op("act", lambda e: e.activation(out=oap, in_=iap, func=func, **kw), reads=rd, writes=[ob])

    def copy(self, eng, ob, oap, iap, rd):
        if eng == "act":
            self.P.op("act", lambda e: e.activation(out=oap, in_=iap, func=AF.Copy), reads=rd, writes=[ob])
        else:
            self.P.op(eng, lambda e: e.tensor_copy(out=oap, in_=iap), reads=rd, writes=[ob])

    def altcopy(self, ob, oap, iap, rd):
        self.alt += 1
        self.copy("act" if self.alt % 2 else "dve", ob, oap, iap, rd)

    def tt(self, ob, oap, a, b, op, rd, eng="dve"):
        self.P.op(eng, lambda e: e.tensor_tensor(out=oap, in0=a, in1=b, op=op), reads=rd, writes=[ob])

    def ts(self, ob, oap, a, s1, s2, op0, op1, rd, eng="dve"):
        if s2 is None:
            self.P.op(eng, lambda e: e.tensor_scalar(out=oap, in0=a, scalar1=s1, scalar2=None, op0=op0),
                      reads=rd, writes=[ob])
        else:
            self.P.op(eng, lambda e: e.tensor_scalar(out=oap, in0=a, scalar1=s1, scalar2=s2, op0=op0, op1=op1),
                      reads=rd, writes=[ob])

    def stt(self, ob, oap, a, s, b, op0, op1, rd, eng="dve"):
        self.P.op(eng, lambda e: e.scalar_tensor_tensor(out=oap, in0=a, scalar=s, in1=b, op0=op0, op1=op1),
                  reads=rd, writes=[ob])

    def load(self, ob, oap, src, sap, q="sp"):
        self.P.dma(q, lambda e: e.dma_start(out=oap, in_=sap), reads=[src], writes=[ob])

    def store(self, dst, dap, sbuf, sap, q="sp"):
        self.P.dma(q, lambda e: e.dma_start(out=dap, in_=sap), reads=[sbuf], writes=[dst])

    def end_phase(self):
        self.P.barrier()
        self.P.emit()
        self.phase_id += 1

    def make_consts(self, es):
        nc, P = self.nc, self.P
        c = {}
        idf = self.sb(es, "idf", [128, 128], F32)
        P.op("pool", lambda e: e.memset(idf.t[:], 1.0), writes=[idf])
        P.op("pool", lambda e: e.affine_select(out=idf.t[:], in_=idf.t[:], pattern=[[-1, 128]],
                                               compare_op=ALU.is_equal, fill=0.0, base=0, channel_multiplier=1),
             reads=[idf], writes=[idf])
        idb = self.sb(es, "idb", [128, 128], BF16)
        self.copy("dve", idb, idb.t[:], idf.t[:], [idf])
        ones = self.sb(es, "onesb", [128, 128], BF16)
        P.op("dve", lambda e: e.memset(ones.t[:], 1.0), writes=[ones])
        nuf = self.sb(es, "nuf", [128, 128], F32)
        P.op("pool", lambda e: e.memset(nuf.t[:], -1.0), writes=[nuf])
        P.op("pool", lambda e: e.affine_select(out=nuf.t[:], in_=nuf.t[:], pattern=[[-1, 128]],
                                               compare_op=ALU.is_ge, fill=0.0, base=0, channel_multiplier=1),
             reads=[nuf], writes=[nuf])
        negu = self.sb(es, "negu", [128, 128], BF16)
        self.copy("dve", negu, negu.t[:], nuf.t[:], [nuf])
        c["idb"], c["ones"], c["negu"], c["idf"] = idb, ones, negu, idf
        return c

    def layer_norm(self, xin, xap, gB, bB, out32, o32ap, out16, o16ap, tmp):
        P = self.P
        st, mv, rs = tmp["st"], tmp["mv"], tmp["rs"]
        for j in range(4):
            P.op("dve", (lambda j: lambda e: e.bn_stats(st.t[:, 6 * j:6 * j + 6], xap[:, 512 * j:512 * j + 512]))(j),
                 reads=[xin], writes=[st])
        P.op("dve", lambda e: e.bn_aggr(mv.t[:], st.t[:]), reads=[st], writes=[mv])
        self.actf(rs, rs.t[:, 0:1], mv.t[:, 1:2], AF.Sqrt, [mv], bias=tmp["eps"].t[:, 0:1])
        P.op("dve", lambda e: e.reciprocal(out=rs.t[:, 0:1], in_=rs.t[:, 0:1]), reads=[rs], writes=[rs])
        self.stt(rs, rs.t[:, 1:2], mv.t[:, 0:1], -1.0, rs.t[:, 0:1], ALU.mult, ALU.mult, [mv, rs])
        y = tmp["y"]
        self.actf(y, y.t[:], xap, AF.Identity, [xin, rs], bias=rs.t[:, 1:2], scale=rs.t[:, 0:1])
        self.tt(y, y.t[:], y.t[:], gB.t[:], ALU.mult, [y, gB])
        if out32 is not None:
            self.tt(out32, o32ap, y.t[:], bB.t[:], ALU.add, [y, bB])
            if out16 is not None:
                self.copy("act", out16, o16ap, o32ap, [out32])
        else:
            self.tt(out16, o16ap, y.t[:], bB.t[:], ALU.add, [y, bB])

    def ln_tmp(self, es, pfx):
        t = {"st": self.sb(es, pfx + "st", [128, 24], F32), "mv": self.sb(es, pfx + "mv", [128, 2], F32),
             "rs": self.sb(es, pfx + "rs", [128, 2], F32), "y": self.sb(es, pfx + "y", [128, D], F32),
             "eps": self.sb(es, pfx + "eps", [128, 1], F32)}
        self.P.op("dve", lambda e: e.memset(t["eps"].t[:], LN_EPS), writes=[t["eps"]])
        return t

    def bcast_load(self, es, name, src, sap):
        b = self.sb(es, name, [128, D], F32)
        self.load(b, b.t[:], src, sap.partition_broadcast(128))
        return b

    def phase0(self, xs, xo, with_ln, lng, lnb, xnT_s, xnT_o, xres):
        with ExitStack() as es:
            C = self.make_consts(es)
            if with_ln:
                gB = self.bcast_load(es, "p0g", lng, lng.t)
                bB = self.bcast_load(es, "p0b", lnb, lnb.t)
                tmp = self.ln_tmp(es, "p0")
            xin = self.sbn(es, "p0x", 2, [128, D], F32)
            y32 = self.sbn(es, "p0y32", 2, [128, D], F32)
            y16 = self.sbn(es, "p0y16", 2, [128, D], BF16)
            xT = self.sbn(es, "p0xT", 2, [128, KC, 512], BF16)
            tps = self.psn(es, "p0tp", 2, [128, D], BF16)
            for (src, ntile, dstT, own) in ((xs, self.S // 128, xnT_s, False), (xo, self.T // 128, xnT_o, True)):
                for ti in range(ntile):
                    xi = xin.next()
                    self.load(xi, xi.t[:], src, src.t[ti * 128:(ti + 1) * 128, :])
                    b16 = y16.next()
                    if with_ln:
                        b32 = y32.next() if own else None
                        self.layer_norm(xi, xi.t[:], gB, bB, b32, b32.t[:] if own else None, b16, b16.t[:], tmp)
                        if own:
                            self.store(xres, xres.t[ti * 128:(ti + 1) * 128, :], b32, b32.t[:], q="pool")
                    else:
                        self.copy("act", b16, b16.t[:], xi.t[:], [xi])
                        if own:
                            self.store(xres, xres.t[ti * 128:(ti + 1) * 128, :], xi, xi.t[:])
                    sub = ti % 4
                    if sub == 0:
                        xTb = xT.next()
                    tpp = tps.next()
                    for k in range(KC):
                        self.tp(tpp, tpp.t[:, k * 128:(k + 1) * 128], b16.t[:, k * 128:(k + 1) * 128], C["idb"], [b16])
                    self.altcopy(xTb, xTb.t[:, :, sub * 128:(sub + 1) * 128],
                                 tpp.t[:].rearrange("p (k t) -> p k t", k=KC), [tpp])
                    if sub == 3:
                        st = ti // 4
                        self.store(dstT, dstT.t[st], xTb, xTb.t[:], q="pool")
            self.end_phase()

    def rope_tables(self, es, pfx):
        r = {"pi": self.sb(es, pfx + "pi", [128, 1], I32), "pf": self.sb(es, pfx + "pf", [128, 1], F32),
             "ang": self.sb(es, pfx + "ang", [128, 192], F32), "tab": self.sb(es, pfx + "tab", [128, 192], F32),
             "tmp": self.sb(es, pfx + "tmp", [128, 192], F32)}
        return r

    def compute_rope(self, r, frB, possrc, posap, tabB=None, tabap=None):
        P = self.P
        MAGIC = 12582912.0
        self.load(r["pi"], r["pi"].t[:], possrc, posap)
        self.copy("dve", r["pf"], r["pf"].t[:], r["pi"].t[:], [r["pi"]])
        ang, tmp, tab = r["ang"], r["tmp"], r["tab"]
        self.ts(ang, ang.t[:], frB.t[:], r["pf"].t[:, 0:1], None, ALU.mult, None, [frB, r["pf"]])
        for (a, b) in ((0, 64), (128, 160)):
            self.ts(ang, ang.t[:, a:b], ang.t[:, a:b], float(np.pi / 2), None, ALU.add, None, [ang])
        self.ts(tmp, tmp.t[:], ang.t[:], float(1 / (2 * np.pi)), MAGIC, ALU.mult, ALU.add, [ang])
        self.ts(tmp, tmp.t[:], tmp.t[:], MAGIC, float(-2 * np.pi), ALU.subtract, ALU.mult, [tmp])
        self.tt(tmp, tmp.t[:], tmp.t[:], ang.t[:], ALU.add, [tmp, ang])
        self.ts(tmp, tmp.t[:], tmp.t[:], float(-np.pi), float(np.pi), ALU.max, ALU.min, [tmp])
        if tabB is None:
            self.actf(tab, tab.t[:], tmp.t[:], AF.Sin, [tmp])
        else:
            self.actf(tabB, tabap, tmp.t[:], AF.Sin, [tmp])

    def rope_apply(self, ps, psap, nh, hd, cosap, sinap, out, oap, t1, t2, extra_scale=None, rd=()):
        h2 = hd // 2
        x = psap.rearrange("p (h t d) -> p h t d", h=nh, t=2)
        o = oap.rearrange("p (h t d) -> p h t d", h=nh, t=2)
        x1, x2 = x[:, :, 0, :], x[:, :, 1, :]
        cb = cosap.unsqueeze(1).to_broadcast([128, nh, h2])
        sb_ = sinap.unsqueeze(1).to_broadcast([128, nh, h2])
        a1 = t1.t[:, 0:nh * h2].rearrange("p (h d) -> p h d", h=nh)
        a2 = t2.t[:, 0:nh * h2].rearrange("p (h d) -> p h d", h=nh)
        rd = list(rd)
        self.tt(t1, a1, x1, cb, ALU.mult, [ps] + rd)
        self.tt(t2, a2, x2, sb_, ALU.mult, [ps] + rd)
        self.tt(out, o[:, :, 0, :], a1, a2, ALU.subtract, [t1, t2])
        self.tt(t1, a1, x2, cb, ALU.mult, [ps] + rd)
        self.tt(t2, a2, x1, sb_, ALU.mult, [ps] + rd)
        self.tt(out, o[:, :, 1, :], a1, a2, ALU.add, [t1, t2])

    def phaseA(self, L, w_in, xnT_s, xs_ap, xnT_o, xo_ap, pos_s, pos_o, frq, sc):
        nc, P = self.nc, self.P
        wv = w_in.t.rearrange("(k p) c -> p k c", p=128)
        NTS, NTO = self.S // 128, self.T // 128
        with ExitStack() as es:
            C = self.make_consts(es)
            frB = self.sb(es, "frB", [128, 192], F32)
            self.load(frB, frB.t[:], frq, frq.t.partition_broadcast(128))
            wch = self.sbn(es, "wch", 3, [128, KC, 512], BF16)
            xTt = self.sbn(es, "xTt", 3, [128, KC, 512], BF16)
            acc = self.psn(es, "acc", 4, [128, 512], F32)
            tpp = self.psn(es, "tpp", 2, [128, 512], BF16)
            rp = self.rope_tables(es, "rp")
            tabS = self.sb(es, "tabS", [128, NTS, 192], F32)
            tabO = self.sb(es, "tabO", [128, NTO, 192], F32)
            t1 = self.sb(es, "rt1", [128, 256], F32)
            t2 = self.sb(es, "rt2", [128, 256], F32)
            roped = self.sbn(es, "roped", 2, [128, 512], BF16)
            tstage = self.sbn(es, "tstage", 2, [128, 4, 512], BF16)
            mstage = self.sbn(es, "mstage", 2, [128, 4, 512], BF16)
            fstage = self.sbn(es, "fstage", 3, [128, 512], BF16)
            wsb = self.sb(es, "wsb", [128, NTO, 16], F32)
            wab = self.sb(es, "wab", [128, NTO, 16], F32)
            wsg = self.sb(es, "wsg", [128, NTO, 16], F32)

            for (tabB, possrc, nt) in ((tabS, pos_s, NTS), (tabO, pos_o, NTO)):
                for ti in range(nt):
                    self.compute_rope(rp, frB, possrc, possrc.t[ti * 128:(ti + 1) * 128, :], tabB, tabB.t[:, ti, :])

            def load_w(c0, w):
                b = wch.next()
                self.load(b, b.t[:, :, 0:w], w_in, wv[:, :, c0:c0 + w], q="pool")
                return b

            def load_x(src, ap):
                xb = xTt.next()
                self.load(xb, xb.t[:], src, ap)
                return xb

            def tok_major(xb, sub, wb, w):
                a = acc.next()
                for k in range(KC):
                    self.mm(a, a.t[:, 0:w], xb.t[:, k, sub * 128:(sub + 1) * 128], wb.t[:, k, 0:w],
                            k == 0, k == KC - 1, [xb, wb])
                return a

            def feat_major(xb, wb, c):
                a = acc.next()
                for k in range(KC):
                    self.mm(a, a.t[:], wb.t[:, k, c * 128:(c + 1) * 128], xb.t[:, k, :], k == 0, k == KC - 1, [xb, wb])
                return a

            def transposes(src16, ncol, stage, sub):
                nb = ncol // 128
                tq = tpp.next()
                for c in range(nb):
                    self.tp(tq, tq.t[:, c * 128:(c + 1) * 128], src16.t[:, c * 128:(c + 1) * 128], C["idb"], [src16])
                self.altcopy(stage, stage.t[:, 0:nb, sub * 128:(sub + 1) * 128],
                             tq.t[:, 0:nb * 128].rearrange("p (c t) -> p c t", c=nb), [tq])

            def roped_chunk(xb, wb, st, tabB, dst, h0, nh, hd, cofs, scale, wfold):
                tok = slice(st * 512, (st + 1) * 512)
                stg = tstage.next()
                for sub in range(4):
                    ti = st * 4 + sub
                    a = tok_major(xb, sub, wb, 512)
                    rb = roped.next()
                    self.rope_apply(a, a.t[:], nh, hd, tabB.t[:, ti, cofs:cofs + hd // 2],
                                    tabB.t[:, ti, cofs + hd // 2:cofs + hd], rb, rb.t[:], t1, t2, rd=[tabB])
                    if scale is not None:
                        self.ts(rb, rb.t[:], rb.t[:], scale, None, ALU.mult, None, [rb])
                    if wfold is not None:
                        r3 = rb.t[:].rearrange("p (h d) -> p h d", h=8)
                        self.tt(rb, r3, r3, wab.t[:, ti, wfold:wfold + 8].unsqueeze(2).to_broadcast([128, 8, 64]),
                                ALU.mult, [rb, wab])
                    transposes(rb, 512, stg, sub)
                self.store(dst, dst.t[h0:h0 + 4, :, tok].rearrange("h p t -> p h t"), stg, stg.t[:])

            chunks = []

            def add(c0, w, body):
                chunks.append((c0, w, body))

            for c in range(2):
                def body(wb, c=c):
                    for st in range(self.NST):
                        xb = load_x(xnT_s, xs_ap(st))
                        roped_chunk(xb, wb, st, tabS, sc["kaT"], 4 * c, 4, 128, 0, None, None)
                add(OFF["ka"] + 512 * c, 512, body)
            for (nm, dst) in (("va", sc["va"]), ("vb", sc["vb"])):
                for c in range(2):
                    def body(wb, c=c, dst=dst):
                        for st in range(self.NST):
                            xb = load_x(xnT_s, xs_ap(st))
                            tok = slice(st * 512, (st + 1) * 512)
                            stg = mstage.next()
                            for sub in range(4):
                                a = tok_major(xb, sub, wb, 512)
                                self.altcopy(stg, stg.t[:, sub, :], a.t[:], [a])
                            for hh_ in range(4):
                                self.store(dst, dst.t[4 * c + hh_, :, st * 4:(st + 1) * 4, :],
                                           stg, stg.t[:, :, hh_ * 128:(hh_ + 1) * 128], q="pool")
                    add(OFF[nm] + 512 * c, 512, body)

            def body_ki(wb):
                for st in range(self.NST):
                    xb = load_x(xnT_s, xs_ap(st))
                    tok = slice(st * 512, (st + 1) * 512)
                    stg = tstage.next()
                    for sub in range(4):
                        ti = st * 4 + sub
                        a = tok_major(xb, sub, wb, 64)
                        rb = roped.next()
                        self.rope_apply(a, a.t[:, 0:64], 1, 64, tabS.t[:, ti, 128:160], tabS.t[:, ti, 160:192], rb, rb.t[:, 0:64],
                                        t1, t2, rd=[tabS])
                        tq = tpp.next()
                        self.tp(tq, tq.t[0:64, 0:128], rb.t[:, 0:64], C["idb"], [rb])
                        self.altcopy(stg, stg.t[0:64, 0, sub * 128:(sub + 1) * 128], tq.t[0:64, 0:128], [tq])
                    self.store(sc["kiT"], sc["kiT"].t[:, tok], stg, stg.t[0:64, 0, :], q="pool")
            add(OFF["ki"], 64, body_ki)
            for c2 in range(2):
                def body(wb, c2=c2):
                    for st in range(self.NST):
                        xb = load_x(xnT_s, xs_ap(st))
                        tok = slice(st * 512, (st + 1) * 512)
                        for c in range(4):
                            a = feat_major(xb, wb, c)
                            fs = fstage.next()
                            self.altcopy(fs, fs.t[:], a.t[:], [a])
                            self.store(sc["kbT"], sc["kbT"].t[4 * c2 + c, :, tok], fs, fs.t[:], q="pool")
                add(OFF["kb"] + 512 * c2, 512, body)

            def body_wi(wb):
                for st in range(self.NSLOT):
                    xb = load_x(xnT_o, xo_ap(st))
                    for sub in range(4):
                        a = tok_major(xb, sub, wb, 16)
                        self.copy("dve", wsb, wsb.t[:, st * 4 + sub, :], a.t[:, 0:16], [a])
                self.ts(wab, wab.t[:], wsb.t[:], -1.0, None, ALU.mult, None, [wsb])
                self.tt(wab, wab.t[:], wab.t[:], wsb.t[:], ALU.max, [wab, wsb])
                self.ts(wsg, wsg.t[:], wsb.t[:], 0.0, None, ALU.is_ge, None, [wsb])
                self.ts(wsg, wsg.t[:], wsg.t[:], 2.0, -1.0, ALU.mult, ALU.add, [wsg])
                self.store(sc["wsgn"], sc["wsgn"].t.rearrange("(s p) c -> p s c", p=128), wsg, wsg.t[:], q="pool")
            add(OFF["wi"], 16, body_wi)
            for c in range(2):
                def body(wb, c=c):
                    for st in range(self.NSLOT):
                        xb = load_x(xnT_o, xo_ap(st))
                        roped_chunk(xb, wb, st, tabO, sc["qaT"], 4 * c, 4, 128, 0, SCALE, None)
                add(OFF["qa"] + 512 * c, 512, body)
            for c in range(2):
                def body(wb, c=c):
                    for st in range(self.NSLOT):
                        xb = load_x(xnT_o, xo_ap(st))
                        roped_chunk(xb, wb, st, tabO, sc["qiT"], 4 * c, 8, 64, 128, None, 8 * c)
                add(OFF["qi"] + 512 * c, 512, body)
            for c2 in range(2):
                def body(wb, c2=c2):
                    for st in range(self.NSLOT):
                        xb = load_x(xnT_o, xo_ap(st))
                        tok = slice(st * 512, (st + 1) * 512)
                        for c in range(4):
                            a = feat_major(xb, wb, c)
                            fs = fstage.next()
                            self.actf(fs, fs.t[:], a.t[:], AF.Copy, [a], scale=SCALE)
                            self.store(sc["qbT"], sc["qbT"].t[4 * c2 + c, :, tok], fs, fs.t[:], q="pool")
                add(OFF["qb"] + 512 * c2, 512, body)
            for (nm, dst) in (("ga", sc["gaT"]), ("gb", sc["gbT"])):
                for c2 in range(4):
                    def body(wb, c2=c2, dst=dst):
                        for st in range(self.NSLOT):
                            xb = load_x(xnT_o, xo_ap(st))
                            tok = slice(st * 512, (st + 1) * 512)
                            for c in range(4):
                                a = feat_major(xb, wb, c)
                                fs = fstage.next()
                                self.actf(fs, fs.t[:], a.t[:], AF.Sigmoid, [a])
                                self.store(dst, dst.t[4 * c2 + c, :, tok], fs, fs.t[:], q="pool")
                    add(OFF[nm] + 512 * c2, 512, body)

            wbs = {0: load_w(chunks[0][0], chunks[0][1])}
            for i, (c0, w, body) in enumerate(chunks):
                if i + 1 < len(chunks):
                    wbs[i + 1] = load_w(chunks[i + 1][0], chunks[i + 1][1])
                body(wbs.pop(i))
            self.end_phase()

    def phaseB1(self, sc, dsa_mask):
        P = self.P
        S = self.S
        if self.debug:
            self.dbg_sc = self.dscr(f"dbg_sc_{self.phase_id}", [self.NSLOT * 4, 128, S], F32)
            self.dbg_thr = self.dscr(f"dbg_thr_{self.phase_id}", [self.NSLOT * 4, 128, 1], F32)
            self.dbg_M = self.dscr(f"dbg_M_{self.phase_id}", [self.NSLOT * 4, 128, S], BF16)
            self.dbg_sc2 = self.dscr(f"dbg_sc2_{self.phase_id}", [self.NSLOT * 4, 128, S], F32)
        with ExitStack() as es:
            C = self.make_consts(es)
            if self.debug:
                self.snap = self.sb(es, "snap", [128, S], F32)
            kiT2 = self.sb(es, "kiT2", [128, S], BF16)
            self.load(kiT2, kiT2.t[0:64, :], sc["kiT"], sc["kiT"].t[:, :])
            self.load(kiT2, kiT2.t[64:128, :], sc["kiT"], sc["kiT"].t[:, :])
            NBmax = 4 * (self.NST)
            qiq = self.sbn(es, "qiq", 2, [128, 8, 128], BF16)
            wsg = self.sbn(es, "wsgq", 2, [128, 16], F32)
            dg = self.sbn(es, "dg", 2, [128, 16, 128], BF16)
            Rb = self.sbn(es, "Rb", 4, [128, 512], BF16)
            scbR = self.sbn(es, "scb", 2, [128, S], F32)
            thr = self.sb(es, "thr", [128, 1], F32)
            mid = self.sb(es, "mid", [128, 1], F32)
            cnt = self.sb(es, "cnt", [128, 1], F32)
            geb = self.sb(es, "geb", [128, 1], F32)
            MbR = self.sbn(es, "Mb", 2, [128, S], BF16)
            MT = self.sb(es, "MT", [128, NBmax, 512], BF16)
            mka = self.sbn(es, "mka", 2, [128, 640], F32)
            kaTh = self.sbn(es, "kaTh", 2, [128, S], BF16)
            vah = self.sbn(es, "vah", 2, [128, NBmax, 128], BF16)
            qaTh = self.sbn(es, "qaTh", 2, [128, 512], BF16)
            Pb = self.sbn(es, "Pb", 3, [128, 512], BF16)
            PMb = self.sbn(es, "PMb", 3, [128, 512], BF16)
            rden = self.sb(es, "rden", [128, 512], F32)
            ost = self.sbn(es, "ost", 2, [128, 512], BF16)
            g = self.psn(es, "g", 2, [128, 512], F32)
            scp = self.psn(es, "scp", 2, [128, 512], F32)
            tq = self.psn(es, "tq", 1, [128, 512], BF16)
            Op = self.ps(es, "Op", [128, 512], F32)
            dp = self.ps(es, "dp", [128, 512], F32)

            for scb_ in scbR.bufs:
                P.op("pool", (lambda scb_: lambda e: e.memset(scb_.t[:], NEG))(scb_), writes=[scb_])
            for i in range(self.NSLOT):
                smax = 2 * i + 1
                NB = 4 * (smax + 1)
                P.op("pool", lambda e: e.memset(MT.t[:], 0.0), writes=[MT])
                for r in range(4):
                    qt = 4 * i + r
                    nkb = 4 * smax + r + 1
                    n = 128 * nkb
                    tok = slice(qt * 128, (qt + 1) * 128)
                    q = qiq.next()
                    self.load(q, q.t[:], sc["qiT"], sc["qiT"].t[:, :, tok].rearrange("h p t -> p h t"))
                    ws = wsg.next()
                    self.load(ws, ws.t[:], sc["wsgn"], sc["wsgn"].t[tok, :])
                    mk = mka.next()
                    self.load(mk, mk.t[:], dsa_mask, dsa_mask.t[i, r])
                    scb = scbR.next()
                    Mb = MbR.next()
                    d = dg.next()
                    for h in range(NIH):
                        self.ts(d, d.t[:, h, :], C["idb"].t[:], ws.t[:, h:h + 1], None, ALU.mult, None, [C["idb"], ws], eng="pool")
                    nch = (n + 511) // 512
                    for c in range(nch):
                        w = min(512, n - 512 * c)
                        ks = slice(512 * c, 512 * c + w)
                        spb = scp.next()
                        pend = None
                        for h in range(NIH):
                            half = (h % 2) * 64
                            lg = g.next()
                            self.mm(lg, lg.t[:, 0:w], q.t[half:half + 64, h // 2, :], kiT2.t[half:half + 64, ks],
                                    True, True, [q, kiT2])
                            Rt = Rb.next()
                            self.actf(Rt, Rt.t[:, 0:w], lg.t[:, 0:w], AF.Relu, [lg])
                            if pend is not None:
                                ph, pR = pend
                                self.mm(spb, spb.t[:, 0:w], d.t[:, ph, :], pR.t[:, 0:w], ph == 0, False, [d, pR])
                            pend = (h, Rt)
                        ph, pR = pend
                        self.mm(spb, spb.t[:, 0:w], d.t[:, ph, :], pR.t[:, 0:w], False, True, [d, pR])
                        self.copy("act", scb, scb.t[:, ks], spb.t[:, 0:w], [spb])
                    self.tt(scb, scb.t[:, n - 640:n], scb.t[:, n - 640:n], mk.t[:], ALU.add, [scb, mk])
                    LO0, W0, NIT = -4096.0, 8192.0, 34
                    if self.debug:
                        self.copy("dve", self.snap, self.snap.t[:, 0:n], scb.t[:, 0:n], [scb])
                        self.store(self.dbg_sc2, self.dbg_sc2.t[qt, :, 0:n], self.snap, self.snap.t[:, 0:n])
                    P.op("dve", lambda e: e.memset(thr.t[:], LO0), writes=[thr])
                    if n > TOPK:
                        for it in range(NIT):
                            hstep = W0 / float(2 ** (it + 1))
                            self.ts(mid, mid.t[:], thr.t[:], hstep, None, ALU.add, None, [thr])
                            P.op("dve", lambda e, Mb=Mb, scb=scb, n=n: e.tensor_scalar(
                                out=Mb.t[:, 0:n], in0=scb.t[:, 0:n], scalar1=mid.t[:, 0:1],
                                scalar2=0.0, op0=ALU.is_ge, op1=ALU.add, accum_out=cnt.t[:, 0:1]),
                                 reads=[scb, mid], writes=[Mb, cnt])
                            self.ts(geb, geb.t[:], cnt.t[:], float(TOPK), None, ALU.is_ge, None, [cnt])
                            self.stt(thr, thr.t[:], geb.t[:], hstep, thr.t[:], ALU.mult, ALU.add, [geb, thr])
                    self.ts(Mb, Mb.t[:, 0:n], scb.t[:, 0:n], thr.t[:, 0:1], None, ALU.is_ge, None, [scb, thr])
                    if self.debug:
                        self.store(self.dbg_sc, self.dbg_sc.t[qt, :, 0:n], scb, scb.t[:, 0:n])
                        self.store(self.dbg_thr, self.dbg_thr.t[qt], thr, thr.t[:])
                        self.store(self.dbg_M, self.dbg_M.t[qt, :, 0:n], Mb, Mb.t[:, 0:n])
                    for j0 in range(0, nkb, 4):
                        nb = min(4, nkb - j0)
                        t = tq.next()
                        for jj in range(nb):
                            j = j0 + jj
                            self.tp(t, t.t[:, jj * 128:(jj + 1) * 128], Mb.t[:, j * 128:(j + 1) * 128], C["idb"], [Mb])
                        self.altcopy(MT, MT.t[:, j0:j0 + nb, r * 128:(r + 1) * 128],
                                     t.t[:, 0:nb * 128].rearrange("p (j t) -> p j t", j=nb), [t])
                tok = slice(i * 512, (i + 1) * 512)
                for h in range(NH):
                    kT = kaTh.next()
                    self.load(kT, kT.t[:, 0:NB * 128], sc["kaT"], sc["kaT"].t[h, :, 0:NB * 128])
                    vh = vah.next()
                    self.load(vh, vh.t[:, 0:NB, :], sc["va"], sc["va"].t[h, :, 0:NB, :])
                    qh = qaTh.next()
                    self.load(qh, qh.t[:], sc["qaT"], sc["qaT"].t[h, :, tok])
                    pend = None
                    for j in range(NB):
                        sT = g.next()
                        self.mm(sT, sT.t[:], kT.t[:, j * 128:(j + 1) * 128], qh.t[:], True, True, [kT, qh])
                        pb = Pb.next()
                        self.actf(pb, pb.t[:], sT.t[:], AF.Exp, [sT])
                        pm = PMb.next()
                        self.tt(pm, pm.t[:], pb.t[:], MT.t[:, j, :], ALU.mult, [pb, MT])
                        if pend is not None:
                            pj, ppm = pend
                            self.mm(Op, Op.t[:], vh.t[:, pj, :], ppm.t[:], pj == 0, False, [vh, ppm])
                            self.mm(dp, dp.t[:], C["ones"].t[:], ppm.t[:], pj == 0, False, [C["ones"], ppm])
                        pend = (j, pm)
                    pj, ppm = pend
                    self.mm(Op, Op.t[:], vh.t[:, pj, :], ppm.t[:], pj == 0, True, [vh, ppm])
                    self.mm(dp, dp.t[:], C["ones"].t[:], ppm.t[:], pj == 0, True, [C["ones"], ppm])
                    P.op("dve", lambda e: e.reciprocal(out=rden.t[:], in_=dp.t[:]), reads=[dp], writes=[rden])
                    o = ost.next()
                    self.tt(o, o.t[:], Op.t[:], rden.t[:], ALU.mult, [Op, rden])
                    self.store(sc["oaT"], sc["oaT"].t[h, :, tok], o, o.t[:], q="pool")
            self.end_phase()

    def phaseB2(self, sc, sb_mask):
        P = self.P
        S = self.S
        with ExitStack() as es:
            C = self.make_consts(es)
            NBmax = 4 * self.NST
            kbTh = self.sbn(es, "kbTh", 2, [128, S], BF16)
            vbh = self.sbn(es, "vbh", 2, [128, NBmax, 128], BF16)
            qbTh = self.sbn(es, "qbTh", 2, [128, 512], BF16)
            msk = self.sbn(es, "sbm", 2, [128, 8, 512], BF16)
            carry = self.sb(es, "carry", [128, 512], F32)
            eb = self.sbn(es, "eb", 2, [128, 512], F32)
            spb = self.sbn(es, "spb", 3, [128, 512], BF16)
            Eb = self.sbn(es, "Eb", 2, [128, 512], F32)
            ab = self.sbn(es, "ab", 3, [128, 512], BF16)
            ost = self.sbn(es, "ostb", 2, [128, 512], BF16)
            zp = self.psn(es, "zp", 3, [128, 512], F32)
            tp_ = self.psn(es, "totp", 2, [128, 512], F32)
            Op = self.ps(es, "Opb", [128, 512], F32)
            for i in range(self.NSLOT):
                smax = 2 * i + 1
                NB = 4 * (smax + 1)
                tok = slice(i * 512, (i + 1) * 512)
                mk = msk.next()
                self.load(mk, mk.t[:], sb_mask, sb_mask.t[i])
                for h in range(NH):
                    kT = kbTh.next()
                    self.load(kT, kT.t[:, 0:NB * 128], sc["kbT"], sc["kbT"].t[h, :, 0:NB * 128])
                    vh = vbh.next()
                    self.load(vh, vh.t[:, 0:NB, :], sc["vb"], sc["vb"].t[h, :, 0:NB, :])
                    qh = qbTh.next()
                    self.load(qh, qh.t[:], sc["qbT"], sc["qbT"].t[h, :, tok])
                    order = list(range(NB - 1, -1, -1))

                    def stage1(j, kT=kT, qh=qh, mk=mk, NB=NB):
                        jb = j - (NB - 8)
                        z = zp.next()
                        self.mm(z, z.t[:], kT.t[:, j * 128:(j + 1) * 128], qh.t[:], True, True, [kT, qh])
                        e_ = eb.next()
                        self.actf(e_, e_.t[:], z.t[:], AF.Exp, [z])
                        sp = spb.next()
                        self.actf(sp, sp.t[:], e_.t[:], AF.Ln, [e_], bias=1.0)
                        if jb >= 0:
                            self.tt(sp, sp.t[:], sp.t[:], mk.t[:, jb, :], ALU.mult, [sp, mk])
                        self.mm(z, z.t[:], C["negu"].t[:], sp.t[:], False, True, [C["negu"], sp])
                        return z, sp

                    cur = stage1(order[0])
                    for idx, j in enumerate(order):
                        z, sp = cur
                        nxt = stage1(order[idx + 1]) if idx + 1 < len(order) else None
                        first = (idx == 0)
                        jb = j - (NB - 8)
                        a = ab.next()
                        if first:
                            self.actf(a, a.t[:], z.t[:], AF.Exp, [z])
                        else:
                            E = Eb.next()
                            self.tt(E, E.t[:], z.t[:], carry.t[:], ALU.subtract, [z, carry])
                            self.actf(a, a.t[:], E.t[:], AF.Exp, [E])
                        if jb >= 0:
                            self.tt(a, a.t[:], a.t[:], mk.t[:, jb, :], ALU.mult, [a, mk])
                        self.mm(Op, Op.t[:], vh.t[:, j, :], a.t[:], first, j == 0, [vh, a])
                        if j > 0:
                            tb = tp_.next()
                            self.mm(tb, tb.t[:], C["ones"].t[:], sp.t[:], True, True, [C["ones"], sp])
                            if first:
                                self.copy("dve", carry, carry.t[:], tb.t[:], [tb])
                            else:
                                self.tt(carry, carry.t[:], carry.t[:], tb.t[:], ALU.add, [carry, tb])
                        cur = nxt
                    o = ost.next()
                    self.copy("act", o, o.t[:], Op.t[:], [Op])
                    self.store(sc["obT"], sc["obT"].t[h, :, tok], o, o.t[:], q="pool")
            self.end_phase()

    def tr16(self, tq, C, src16, dst, dst_sub):
        for k0 in range(0, KC, 8):
            t = tq.next()
            for k in range(8):
                self.tp(t, t.t[:, k * 128:(k + 1) * 128], src16.t[:, (k0 + k) * 128:(k0 + k + 1) * 128], C["idb"], [src16])
            self.altcopy(dst, dst.t[:, k0:k0 + 8, dst_sub], t.t[:].rearrange("p (k t) -> p k t", k=8), [t])

    def phaseC1(self, W, sc, xres, xmres, xmT_d):
        P = self.P
        with ExitStack() as es:
            C = self.make_consts(es)
            gM = self.bcast_load(es, "gM", W["lnm_g"], W["lnm_g"].t)
            bM = self.bcast_load(es, "bM", W["lnm_b"], W["lnm_b"].t)
            tmp = self.ln_tmp(es, "pc")
            w4 = self.sbn(es, "w4", 4, [128, NH, 256], BF16)
            wd = self.sbn(es, "wo2", 2, [128, 2, D], BF16)
            oT = self.sbn(es, "oT", 2, [128, NH, 512], BF16)
            gT = self.sbn(es, "gT", 2, [128, 4, 512], BF16)
            m1 = self.sb(es, "m1", [128, 512], F32)
            mT = self.sb(es, "mT", [128, KC, 512], BF16)
            accb = self.sb(es, "accb", [128, 4, D], F32)
            x16 = self.sbn(es, "x16", 2, [128, D], BF16)
            xmT = self.sb(es, "xmT", [128, KC, 512], BF16)
            bigs = [self.ps(es, f"bigc{c}", [128, 512], F32) for c in range(4)]
            pq = self.psn(es, "pq", 3, [128, 512], F32)
            tq = self.psn(es, "tq", 1, [128, 1024], BF16)
            for ti in range(self.NSLOT):
                tok = slice(ti * 512, (ti + 1) * 512)
                oa = oT.next()
                self.load(oa, oa.t[:], sc["oaT"], sc["oaT"].t[:, :, tok].rearrange("h p t -> p h t"))
                ob = oT.next()
                self.load(ob, ob.t[:], sc["obT"], sc["obT"].t[:, :, tok].rearrange("h p t -> p h t"))
                for sub in range(4):
                    self.load(accb, accb.t[:, sub, :], xres, xres.t[ti * 512 + sub * 128: ti * 512 + (sub + 1) * 128, :])
                for fg in range(4):
                    ga = gT.next()
                    self.load(ga, ga.t[:], sc["gaT"], sc["gaT"].t[4 * fg:4 * fg + 4, :, tok].rearrange("c p t -> p c t"))
                    gb = gT.next()
                    self.load(gb, gb.t[:], sc["gbT"], sc["gbT"].t[4 * fg:4 * fg + 4, :, tok].rearrange("c p t -> p c t"))
                    for half in range(2):
                        c0 = fg * 512 + half * 256
                        wa = w4.next()
                        self.load(wa, wa.t[:], W["pa"], W["pa"].t[:, c0:c0 + 256].rearrange("(h p) c -> p h c", p=128), q="pool")
                        wb_ = w4.next()
                        self.load(wb_, wb_.t[:], W["pb"], W["pb"].t[:, c0:c0 + 256].rearrange("(h p) c -> p h c", p=128), q="pool")
                        for c in range(2):
                            fc = fg * 4 + half * 2 + c
                            gi = half * 2 + c
                            pa = pq.next()
                            for h in range(NH):
                                self.mm(pa, pa.t[:], wa.t[:, h, c * 128:(c + 1) * 128], oa.t[:, h, :], h == 0, h == NH - 1, [wa, oa])
                            pb = pq.next()
                            for h in range(NH):
                                self.mm(pb, pb.t[:], wb_.t[:, h, c * 128:(c + 1) * 128], ob.t[:, h, :], h == 0, h == NH - 1, [wb_, ob])
                            self.tt(m1, m1.t[:], pa.t[:], ga.t[:, gi, :], ALU.mult, [pa, ga])
                            self.tt(mT, mT.t[:, fc, :], pb.t[:], gb.t[:, gi, :], ALU.mult, [pb, gb])
                            self.tt(mT, mT.t[:, fc, :], mT.t[:, fc, :], m1.t[:], ALU.add, [mT, m1])
                for sub in range(4):
                    self.actf(accb, accb.t[:, sub, :], accb.t[:, sub, :], AF.Copy, [accb], scale=ALPHA)
                for kg in range(8):
                    wo = wd.next()
                    self.load(wo, wo.t[:], W["wo"], W["wo"].t[kg * 256:(kg + 1) * 256, :].rearrange("(k p) c -> p k c", p=128), q="pool")
                    for sub in range(4):
                        for c in range(4):
                            bg = bigs[c]
                            cs = slice(c * 512, (c + 1) * 512)
                            for k in range(2):
                                self.mm(bg, bg.t[:], mT.t[:, kg * 2 + k, sub * 128:(sub + 1) * 128],
                                        wo.t[:, k, cs], k == 0, k == 1, [mT, wo])
                            self.tt(accb, accb.t[:, sub, cs], accb.t[:, sub, cs], bg.t[:], ALU.add, [accb, bg])
                for sub in range(4):
                    b16 = x16.next()
                    self.layer_norm(accb, accb.t[:, sub, :], gM, bM, accb, accb.t[:, sub, :], b16, b16.t[:], tmp)
                    self.store(xmres, xmres.t[ti * 512 + sub * 128: ti * 512 + (sub + 1) * 128, :], accb, accb.t[:, sub, :])
                    self.tr16(tq, C, b16, xmT, slice(sub * 128, (sub + 1) * 128))
                self.store(xmT_d, xmT_d.t[ti], xmT, xmT.t[:])
            self.end_phase()

    def phaseC2(self, W, xmres, xmT_d, facc, moe):
        P = self.P
        dff = D_EXP if moe else D_FF
        NF = dff // 128
        nexp = NE if moe else 1
        PIECE = 4
        TT = min(1024, self.T)
        NH2 = TT // 512
        NSUB = TT // 128
        with ExitStack() as es:
            C = self.make_consts(es)
            w8 = self.sbn(es, "w8", 3, [128, KC, 256], BF16)
            wd = self.sbn(es, "wd2", 2, [128, 4, D], BF16)
            accb = self.sb(es, "accb2", [128, NSUB, D], F32)
            xT = self.sb(es, "xmT2", [128, KC, TT], BF16)
            actT = self.sbn(es, "actT", 2, [128, PIECE, TT], BF16)
            sg = self.sbn(es, "sg", 2, [128, 512], F32)
            x16 = self.sb(es, "x16b", [128, D], BF16)
            bigs = [self.ps(es, f"big{c}", [128, 512], F32) for c in range(4)]
            pq = self.psn(es, "pq2", 3, [128, 512], F32)
            tq = self.psn(es, "tq2", 1, [128, 1024], BF16)
            if moe:
                xlf = self.sb(es, "xlf", [128, D], F32)
                if TT >= 1024:
                    a0, a1 = actT.bufs[0], actT.bufs[1]
                    xhT = _View(a0, a0.t[:, 0:2, :].rearrange("p a (b t) -> p (a b) t", t=128))
                    xlT = _View(a0, a0.t[:, 2:4, :].rearrange("p a (b t) -> p (a b) t", t=128))
                    xlo = _View(a1, a1.t[:, 0:2, :].rearrange("p a t -> p (a t)"))
                else:
                    b1_, b2_, b3_ = (self.sb(es, "xhTs", [128, KC, 128], BF16), self.sb(es, "xlTs", [128, KC, 128], BF16),
                                     self.sb(es, "xlos", [128, D], BF16))
                    xhT, xlT, xlo = _View(b1_, b1_.t[:]), _View(b2_, b2_.t[:]), _View(b3_, b3_.t[:])
                rwf = self.sb(es, "rwf", [128, KC, NE], F32)
                rwh = self.sb(es, "rwh", [128, KC, NE], BF16)
                rwl = self.sb(es, "rwl", [128, KC, NE], BF16)
                rwt = self.sb(es, "rwt", [128, KC, NE], F32)
                self.load(rwf, rwf.t[:], W["rw"], W["rw"].t.rearrange("(k p) e -> p k e", p=128))
                self.copy("dve", rwh, rwh.t[:], rwf.t[:], [rwf])
                self.copy("dve", rwt, rwt.t[:], rwh.t[:], [rwh])
                self.tt(rwt, rwt.t[:], rwf.t[:], rwt.t[:], ALU.subtract, [rwf, rwt])
                self.copy("dve", rwl, rwl.t[:], rwt.t[:], [rwt])
                lgs = self.sb(es, "lgs", [128, NE], F32)
                l8 = self.sb(es, "l8", [128, 8], F32)
                gts = self.sb(es, "gts", [128, 4], F32)
                eq = self.sb(es, "eq", [128, 2, NE], F32)
                comb = self.sb(es, "comb", [128, NSUB, NE], F32)
            for ti in range(self.T // TT):
                for hh in range(NH2):
                    self.load(xT, xT.t[:, :, hh * 512:(hh + 1) * 512], xmT_d, xmT_d.t[ti * NH2 + hh])
                for sub in range(NSUB):
                    rows = slice(ti * TT + sub * 128, ti * TT + (sub + 1) * 128)
                    self.load(accb, accb.t[:, sub, :], xmres, xmres.t[rows, :])
                    if moe:
                        self.copy("act", x16, x16.t[:], accb.t[:, sub, :], [accb])
                        self.copy("dve", xlf, xlf.t[:], x16.t[:], [x16])
                        self.tt(xlo, xlo.t, accb.t[:, sub, :], xlf.t[:], ALU.subtract, [accb, xlf])
                        for (srcb, sap, dstT) in ((x16, x16.t, xhT), (xlo, xlo.t, xlT)):
                            for k0 in range(0, KC, 8):
                                t = tq.next()
                                for k in range(8):
                                    self.tp(t, t.t[:, k * 128:(k + 1) * 128], sap[:, (k0 + k) * 128:(k0 + k + 1) * 128], C["idb"], [srcb])
                                self.altcopy(dstT, dstT.t[:, k0:k0 + 8, :], t.t[:].rearrange("p (k t) -> p k t", k=8), [t])
                        lp = pq.next()
                        n3 = 3 * KC
                        idx = 0
                        for (a_, b_) in ((xhT, rwh), (xlT, rwh), (xhT, rwl)):
                            for k in range(KC):
                                self.mm(lp, lp.t[:, 0:NE], a_.t[:, k, :], b_.t[:, k, :], idx == 0, idx == n3 - 1, [a_, b_])
                                idx += 1
                        self.copy("dve", lgs, lgs.t[:], lp.t[:, 0:NE], [lp])
                        P.op("dve", lambda e: e.max(out=l8.t[:], in_=lgs.t[:]), reads=[lgs], writes=[l8])
                        self.tt(gts, gts.t[:, 0:1], l8.t[:, 0:1], l8.t[:, 1:2], ALU.subtract, [l8])
                        self.actf(gts, gts.t[:, 1:2], gts.t[:, 0:1], AF.Sigmoid, [gts])
                        self.ts(gts, gts.t[:, 2:3], gts.t[:, 1:2], -1.0, 1.0, ALU.mult, ALU.add, [gts])
                        self.ts(eq, eq.t[:, 0, :], lgs.t[:], l8.t[:, 0:1], None, ALU.is_equal, None, [lgs, l8])
                        self.ts(eq, eq.t[:, 0, :], eq.t[:, 0, :], gts.t[:, 1:2], None, ALU.mult, None, [eq, gts])
                        self.ts(eq, eq.t[:, 1, :], lgs.t[:], l8.t[:, 1:2], None, ALU.is_equal, None, [lgs, l8])
                        self.ts(eq, eq.t[:, 1, :], eq.t[:, 1, :], gts.t[:, 2:3], None, ALU.mult, None, [eq, gts])
                        self.tt(comb, comb.t[:, sub, :], eq.t[:, 0, :], eq.t[:, 1, :], ALU.add, [eq])
                    self.actf(accb, accb.t[:, sub, :], accb.t[:, sub, :], AF.Copy, [accb], scale=ALPHA)
                for ex in range(nexp):
                    if moe:
                        Wg, Wu, Wdn = W["mg"].t[ex], W["mu"].t[ex], W["md"].t[ex]
                        wgB, wuB, wdB = W["mg"], W["mu"], W["md"]
                    else:
                        Wg, Wu, Wdn = W["wg"].t, W["wu"].t, W["wd"].t
                        wgB, wuB, wdB = W["wg"], W["wu"], W["wd"]
                    Wgv = Wg.rearrange("(k p) c -> p k c", p=128)
                    Wuv = Wu.rearrange("(k p) c -> p k c", p=128)
                    for p0 in range(0, NF, PIECE):
                        aT = actT.next()
                        for f2 in range(PIECE // 2):
                            c0 = (p0 + 2 * f2) * 128
                            wgc = w8.next()
                            self.load(wgc, wgc.t[:], wgB, Wgv[:, :, c0:c0 + 256], q="pool")
                            wuc = w8.next()
                            self.load(wuc, wuc.t[:], wuB, Wuv[:, :, c0:c0 + 256], q="pool")
                            for c in range(2):
                                for hh in range(NH2):
                                    ts_ = slice(hh * 512, (hh + 1) * 512)
                                    gp = pq.next()
                                    for k in range(KC):
                                        self.mm(gp, gp.t[:], wgc.t[:, k, c * 128:(c + 1) * 128], xT.t[:, k, ts_], k == 0, k == KC - 1, [wgc, xT])
                                    up = pq.next()
                                    for k in range(KC):
                                        self.mm(up, up.t[:], wuc.t[:, k, c * 128:(c + 1) * 128], xT.t[:, k, ts_], k == 0, k == KC - 1, [wuc, xT])
                                    s_ = sg.next()
                                    self.actf(s_, s_.t[:], gp.t[:], AF.Silu, [gp])
                                    self.tt(aT, aT.t[:, 2 * f2 + c, ts_], s_.t[:], up.t[:], ALU.mult, [s_, up])
                        wdc = wd.next()
                        r0 = p0 * 128
                        self.load(wdc, wdc.t[:], wdB, Wdn[r0:r0 + 512, :].rearrange("(k p) c -> p k c", p=128), q="pool")
                        for sub in range(NSUB):
                            for c in range(4):
                                bg = bigs[c]
                                for k in range(PIECE):
                                    self.mm(bg, bg.t[:], aT.t[:, k, sub * 128:(sub + 1) * 128],
                                            wdc.t[:, k, c * 512:(c + 1) * 512], k == 0, k == PIECE - 1, [aT, wdc])
                                cs = slice(c * 512, (c + 1) * 512)
                                if moe:
                                    self.stt(accb, accb.t[:, sub, cs], bg.t[:], comb.t[:, sub, ex:ex + 1], accb.t[:, sub, cs],
                                             ALU.mult, ALU.add, [bg, comb, accb])
                                else:
                                    self.tt(accb, accb.t[:, sub, cs], accb.t[:, sub, cs], bg.t[:], ALU.add, [accb, bg])
                for sub in range(NSUB):
                    rows = slice(ti * TT + sub * 128, ti * TT + (sub + 1) * 128)
                    self.store(facc, facc.t[rows, :], accb, accb.t[:, sub, :])
            self.end_phase()

    def phaseC3(self, W, facc, out_res, out_T):
        with ExitStack() as es:
            C = self.make_consts(es)
            gF = self.bcast_load(es, "gF", W["lnf_g"], W["lnf_g"].t)
            bF = self.bcast_load(es, "bF", W["lnf_b"], W["lnf_b"].t)
            tmp = self.ln_tmp(es, "pc3")
            xin = self.sbn(es, "c3x", 3, [128, D], F32)
            x16 = self.sbn(es, "c3x16", 2, [128, D], BF16)
            xT = self.sbn(es, "c3xT", 2, [128, KC, 512], BF16)
            tq = self.psn(es, "tq3", 2, [128, 1024], BF16)
            for ti in range(self.T // 128):
                rows = slice(ti * 128, (ti + 1) * 128)
                xi = xin.next()
                self.load(xi, xi.t[:], facc, facc.t[rows, :])
                sub = ti % 4
                if out_T is None:
                    self.layer_norm(xi, xi.t[:], gF, bF, xi, xi.t[:], None, None, tmp)
                else:
                    b16 = x16.next()
                    self.layer_norm(xi, xi.t[:], gF, bF, xi, xi.t[:], b16, b16.t[:], tmp)
                    if sub == 0:
                        xTb = xT.next()
                    self.tr16(tq, C, b16, xTb, slice(sub * 128, (sub + 1) * 128))
                    if sub == 3:
                        self.store(out_T, out_T.t[ti // 4], xTb, xTb.t[:], q="pool")
                self.store(out_res, out_res.t[rows, :], xi, xi.t[:], q="pool")
            self.end_phase()

    def scratch(self, L):
        S, T = self.S, self.T
        p = f"L{L}_"
        return {
            "kaT": self.dscr(p + "kaT", [NH, 128, S], BF16), "kbT": self.dscr(p + "kbT", [NH, 128, S], BF16),
            "kiT": self.dscr(p + "kiT", [DI, S], BF16),
            "va": self.dscr(p + "va", [NH, 128, S // 128, DH], BF16), "vb": self.dscr(p + "vb", [NH, 128, S // 128, DH], BF16),
            "qaT": self.dscr(p + "qaT", [NH, 128, T], BF16), "qbT": self.dscr(p + "qbT", [NH, 128, T], BF16),
            "qiT": self.dscr(p + "qiT", [8, 128, T], BF16), "wsgn": self.dscr(p + "wsgn", [T, NIH], F32),
            "gaT": self.dscr(p + "gaT", [KC, 128, T], BF16), "gbT": self.dscr(p + "gbT", [KC, 128, T], BF16),
            "oaT": self.dscr(p + "oaT", [NH, 128, T], BF16), "obT": self.dscr(p + "obT", [NH, 128, T], BF16),
        }

    def layer_weights(self, L):
        p = f"l{L}_"
        W = {"w_in": self.din(p + "w_in", [D, IN_COLS], F32), "pa": self.din(p + "pa", [NH * DH, D], F32),
             "pb": self.din(p + "pb", [NH * DH, D], F32), "wo": self.din(p + "wo", [D, D], F32),
             "lnm_g": self.din(p + "lnm_g", [D], F32), "lnm_b": self.din(p + "lnm_b", [D], F32),
             "lnf_g": self.din(p + "lnf_g", [D], F32), "lnf_b": self.din(p + "lnf_b", [D], F32)}
        if L % 2 == 0:
            W.update(wg=self.din(p + "wg", [D, D_FF], F32), wu=self.din(p + "wu", [D, D_FF], F32),
                     wd=self.din(p + "wd", [D_FF, D], F32))
        else:
            W.update(rw=self.din(p + "rw", [D, NE], F32), mg=self.din(p + "mg", [NE, D, D_EXP], F32),
                     mu=self.din(p + "mu", [NE, D, D_EXP], F32), md=self.din(p + "md", [NE, D_EXP, D], F32))
        return W

    def build(self):
        S, T = self.S, self.T
        nc = self.nc
        with self.ges:
            xs = self.din("xs", [S, D], F32)
            xo = self.din("xo", [T, D], F32)
            pos_s = self.din("pos_s", [S, 1], I32)
            pos_o = self.din("pos_o", [T, 1], I32)
            frq = self.din("frq", [192], F32)
            dsa_mask = self.din("dsa_mask", [self.NSLOT, 4, 128, 640], F32)
            sb_mask = self.din("sb_mask", [self.NSLOT, 128, 8, 512], BF16)
            y = Buf(nc.dram_tensor("y", [T, D], F32, kind="ExternalOutput").ap())
            xnT_s = self.dscr("xnT_s", [self.NST, 128, KC, 512], BF16)
            xnT_o = self.dscr("xnT_o", [self.NSLOT, 128, KC, 512], BF16)
            xres = self.dscr("xres", [T, D], F32)
            first = self.layers[0]
            if first == 0:
                lng = self.din("ln_in_g", [D], F32)
                lnb = self.din("ln_in_b", [D], F32)
                self.phase0(xs, xo, True, lng, lnb, xnT_s, xnT_o, xres)
            else:
                self.phase0(xs, xo, False, None, None, xnT_s, xnT_o, xres)
            xs_src, xs_ap = xnT_s, (lambda st: xnT_s.t[st])
            xo_src, xo_ap = xnT_o, (lambda i: xnT_o.t[i])
            nl = len(self.layers)
            for li, L in enumerate(self.layers):
                last = (li == nl - 1)
                W = self.layer_weights(L)
                sc = self.scratch(L)
                self.phaseA(L, W["w_in"], xs_src, xs_ap, xo_src, xo_ap, pos_s, pos_o, frq, sc)
                self.phaseB1(sc, dsa_mask)
                self.phaseB2(sc, sb_mask)
                xmres = self.dscr(f"L{L}_xmres", [T, D], F32)
                xmT_d = self.dscr(f"L{L}_xmT", [self.NSLOT, 128, KC, 512], BF16)
                self.phaseC1(W, sc, xres, xmres, xmT_d)
                facc = self.dscr(f"L{L}_facc", [T, D], F32)
                self.phaseC2(W, xmres, xmT_d, facc, moe=(L % 2 == 1))
                if last:
                    self.phaseC3(W, facc, y, None)
                else:
                    NSL = self.NSLOT
                    ag_src = Buf(nc.dram_tensor(f"ag_src{L}", [NSL, 128, KC * 512], BF16, kind="Internal").ap())
                    ag_dst = Buf(nc.dram_tensor(f"ag_dst{L}", [NSL, 256, KC * 512], BF16, kind="Internal").ap())
                    src_v = ag_src.t.rearrange("s p (k t) -> s p k t", k=KC)
                    self.phaseC3(W, facc, xres, _View(ag_src, src_v))
                    groups = [[2 * b, 2 * b + 1] for b in range(self.ncores // 2)]
                    for i in range(NSL):
                        self.P.coll((lambda i: lambda e: e.collective_compute(
                            "AllGather", ALU.bypass, replica_groups=groups,
                            ins=[ag_src.t[i].opt()], outs=[ag_dst.t[i].opt()]))(i),
                            reads=[ag_src], writes=[ag_dst])
                    self.end_phase()
                    where = {}
                    for p in range(2):
                        for i, st in enumerate(own_tiles(self.NST, p)):
                            where[st] = (i, p)
                    dst_v = ag_dst.t.rearrange("s (r p) (k t) -> s r p k t", r=2, k=KC)
                    xs_src, xs_ap = ag_dst, (lambda st, dst_v=dst_v, where=where: dst_v[where[st][0], where[st][1]])
                    xo_src, xo_ap = ag_src, (lambda i, src_v=src_v: src_v[i])
        return nc


def own_tiles(NST, p):
    out = []
    for i in range(NST // 2):
        a, b = 2 * i, 2 * i + 1
        if i % 2 == 1:
            a, b = b, a
        out.append(a if p == 0 else b)
    return out


def host_consts(S, p):
    NST = S // 512
    own = own_tiles(NST, p)
    nsl = len(own)
    dsa = np.zeros((nsl, 4, 128, 640), np.float32)
    sbm = np.zeros((nsl, 128, 8, 512), np.float32)
    for i, st in enumerate(own):
        smax = 2 * i + 1
        NB = 4 * (smax + 1)
        for r in range(4):
            n = 128 * (4 * smax + r + 1)
            s = (n - 640) + np.arange(640)
            t = 512 * st + 128 * r + np.arange(128)
            adm = (s[None, :] // 64) <= (t[:, None] // 64)
            dsa[i, r] = np.where(adm, 0.0, NEG)
        t = 512 * st + np.arange(512)
        for jb in range(8):
            s = 128 * (NB - 8 + jb) + np.arange(128)
            sbm[i, :, jb, :] = (s[:, None] < t[None, :]).astype(np.float32)
    frh = (10000.0 ** (-np.arange(0, DH, 2, dtype=np.float32) / DH)).astype(np.float32)
    fri = (10000.0 ** (-np.arange(0, DI, 2, dtype=np.float32) / DI)).astype(np.float32)
    frq = np.concatenate([frh, frh, fri, fri]).astype(np.float32)
    return dsa, sbm.astype(ml_dtypes.bfloat16), frq, own


_NC_CACHE = {}


def get_nc(S, layers, debug=False, ncores=8):
    key = (S, tuple(layers), debug, ncores)
    if key not in _NC_CACHE:
        _NC_CACHE[key] = Builder(S, list(layers), debug, ncores).build()
    return _NC_CACHE[key]


def layer_inputs(inp, L):
    p = f"l{L}_"
    m = {p + "w_in": inp["w_in"][L], p + "pa": inp["w_proj_a"][L], p + "pb": inp["w_proj_b"][L],
         p + "wo": inp["w_out"][L], p + "lnm_g": inp["ln_mix_g"][L], p + "lnm_b": inp["ln_mix_b"][L],
         p + "lnf_g": inp["ln_ffn_g"][L], p + "lnf_b": inp["ln_ffn_b"][L]}
    i = L // 2
    if L % 2 == 0:
        m.update({p + "wg": inp["ffn_w_gate"][i], p + "wu": inp["ffn_w_up"][i], p + "wd": inp["ffn_w_down"][i]})
    else:
        m.update({p + "rw": inp["router_w"][i], p + "mg": inp["moe_w_gate"][i], p + "mu": inp["moe_w_up"][i],
                  p + "md": inp["moe_w_down"][i]})
    return {k: np.ascontiguousarray(v) for k, v in m.items()}


def run_layers(xfull, inp, layers, debug=False):
    B, S, _ = xfull.shape
    nc = get_nc(S, layers, debug, 2 * B)
    pos = np.asarray(inp["positions"]).astype(np.int32)
    in_maps = []
    owns = []
    wmap = {}
    for L in layers:
        wmap.update(layer_inputs(inp, L))
    if layers[0] == 0:
        wmap["ln_in_g"] = np.ascontiguousarray(inp["ln_in_g"])
        wmap["ln_in_b"] = np.ascontiguousarray(inp["ln_in_b"])
    for c in range(2 * B):
        b, p = c // 2, c % 2
        dsa, sbm, frq, own = host_consts(S, p)
        rows = np.concatenate([np.arange(st * 512, (st + 1) * 512) for st in own])
        owns.append(rows)
        m = {"xs": np.ascontiguousarray(xfull[b]), "xo": np.ascontiguousarray(xfull[b][rows]),
             "pos_s": np.ascontiguousarray(pos[b].reshape(S, 1)), "pos_o": np.ascontiguousarray(pos[b][rows].reshape(-1, 1)),
             "frq": frq, "dsa_mask": dsa, "sb_mask": sbm}
        m.update(wmap)
        in_maps.append(m)
    res = run_bass_kernel_spmd(nc, in_maps, core_ids=list(range(2 * B)))
    out = np.zeros((B, S, D), np.float32)
    for c in range(2 * B):
        out[c // 2][owns[c]] = res.results[c]["y"]
    return out, res


def kernel(**inputs):
    inp = {k: np.asarray(v) for k, v in inputs.items()}
    x = inp["x"].astype(np.float32)
    x2, _ = run_layers(x, inp, [0, 1])
    return x2
```
